# Optimizing a Trainium2 kernel written in Bass

```python
import jax, jax.numpy as jnp
from jax import lax
import numpy as np

D_MODEL = 1024
BATCH = 32
SEQ = 2048
DEPTH = 2

HG_HEADS = 4
HG_DK = 128
HG_DV = 128
HG_CHUNK = 32
FOX_HEADS = 8
FOX_DH = 64
DIFF_HEADS = 4
DIFF_DH = 64
DIFF_DV = 2 * DIFF_DH
N_DIFF_MAPS = 2
BRANCH_WIDTH = 512
N_BRANCHES = 3
Q_BLOCK = 128
ROPE_THETA = 500000.0
ROPE_DIM = DIFF_DH // 4
N_GROUPS = 4
EXPERTS_PER_GROUP = 8
N_EXPERTS = N_GROUPS * EXPERTS_PER_GROUP
TOP_K_IN_GROUP = 2
D_EXPERT = 512
ALPHA = (2 * DEPTH) ** 0.25
BETA = (8 * DEPTH) ** -0.25
LN_EPS = 1e-5
RMS_EPS = 1e-6
NEG_INF = -1e30
EXP_CLAMP = 60.0
MAX_POS_OFFSET = 4096
IN_SPLITS = (HG_HEADS * HG_DK, HG_HEADS * HG_DK, HG_HEADS * HG_DV, HG_HEADS * HG_DV,
             FOX_HEADS * FOX_DH, FOX_HEADS * FOX_DH, FOX_HEADS * FOX_DH, FOX_HEADS,
             DIFF_HEADS * N_DIFF_MAPS * DIFF_DH, DIFF_HEADS * N_DIFF_MAPS * DIFF_DH, DIFF_HEADS * DIFF_DV,
             N_BRANCHES * D_MODEL)
N_IN = sum(IN_SPLITS)

kernel_name = "hgrn2_fox_diffattn_gated_merge_hmoe_deepnorm"


def layer_norm(x, g, b):
    xf = x.astype(jnp.float32)
    mu = jnp.mean(xf, axis=-1, keepdims=True)
    var = jnp.mean(jnp.square(xf - mu), axis=-1, keepdims=True)
    y = (xf - mu) * lax.rsqrt(var + LN_EPS) * g.astype(jnp.float32) + b.astype(jnp.float32)
    return y.astype(x.dtype)


def rms_norm(x, g):
    xf = x.astype(jnp.float32)
    return xf * lax.rsqrt(jnp.mean(jnp.square(xf), axis=-1, keepdims=True) + RMS_EPS) * g.astype(jnp.float32)


def partial_rope(t, cos, sin):
    half = ROPE_DIM // 2
    c = cos[:, :, None, None, :]
    s = sin[:, :, None, None, :]
    tf = t[..., :ROPE_DIM].astype(jnp.float32)
    t1, t2 = tf[..., :half], tf[..., half:]
    rot = jnp.concatenate([t1 * c - t2 * s, t2 * c + t1 * s], axis=-1).astype(t.dtype)
    return jnp.concatenate([rot, t[..., ROPE_DIM:]], axis=-1)


def causal_block_attention(q, k, v, log_f=None):
    _, S, _, _, dk = q.shape
    scale = dk ** -0.5
    c = None if log_f is None else jnp.swapaxes(jnp.cumsum(log_f.astype(jnp.float32), axis=1), 1, 2)
    outs = []
    for blk in range(S // Q_BLOCK):
        q_lo, q_hi = blk * Q_BLOCK, (blk + 1) * Q_BLOCK
        s = jnp.einsum("bqhmd,bkhmd->bhmqk", q[:, q_lo:q_hi], k[:, :q_hi]).astype(jnp.float32) * scale
        if c is not None:
            s = s + (c[:, :, q_lo:q_hi, None] - c[:, :, None, :q_hi])[:, :, None]
        causal = (q_lo + jnp.arange(Q_BLOCK))[:, None] >= jnp.arange(q_hi)[None, :]
        s = jnp.where(causal, s, NEG_INF)
        p = jax.nn.softmax(s, axis=-1).astype(v.dtype)
        outs.append(jnp.einsum("bhmqk,bkhe->bqhme", p, v[:, :q_hi]))
    return jnp.concatenate(outs, axis=1)


def chunked_gated_recurrence(q, k, v, log_f):
    B, S, H, DK = q.shape
    DV = v.shape[-1]
    n_chunks = S // HG_CHUNK

    def to_chunks(t):
        return t.reshape(B, n_chunks, HG_CHUNK, H, t.shape[-1]).transpose(1, 0, 3, 2, 4)

    causal = jnp.tril(jnp.ones((HG_CHUNK, HG_CHUNK), dtype=bool))

    def step(state, inp):
        qc, kc, vc, gc = inp
        b = jnp.cumsum(gc, axis=2)
        o_inter = jnp.einsum("bhtc,bhcv->bhtv", qc * jnp.exp(b), state)
        rel = jnp.minimum(b[:, :, :, None, :] - b[:, :, None, :, :], 0.0)
        decay = jnp.where(causal[:, :, None], jnp.exp(rel), 0.0)
        scores = jnp.einsum("bhtc,bhsc,bhtsc->bhts", qc, kc, decay)
        o_intra = jnp.einsum("bhts,bhsv->bhtv", scores, vc)
        b_last = b[:, :, -1, :]
        k_dec = kc * jnp.exp(b_last[:, :, None, :] - b)
        state = state * jnp.exp(b_last)[..., None] + jnp.einsum("bhsc,bhsv->bhcv", k_dec, vc)
        return state, o_inter + o_intra

    state0 = jnp.zeros((B, H, DK, DV), jnp.float32)
    _, o = lax.scan(step, state0, (to_chunks(q), to_chunks(k), to_chunks(v), to_chunks(log_f)))
    return o.transpose(1, 0, 3, 2, 4).reshape(B, S, H, DV)


def hgrn2_branch(q_lin, f_lin, i_lin, g_lin, lb, norm_g):
    B, S, _ = q_lin.shape
    z = f_lin.astype(jnp.float32).reshape(B, S, HG_HEADS, HG_DK)
    lbh = lb.astype(jnp.float32).reshape(HG_HEADS, HG_DK)
    log_f = jax.nn.log_sigmoid(z) + jnp.log1p(lbh * jnp.exp(jnp.minimum(-z, EXP_CLAMP)))
    k = (1.0 - lbh) * jax.nn.sigmoid(-z)
    q = jax.nn.silu(q_lin.astype(jnp.float32)).reshape(B, S, HG_HEADS, HG_DK)
    v = i_lin.astype(jnp.float32).reshape(B, S, HG_HEADS, HG_DV)
    o = chunked_gated_recurrence(q, k, v, log_f)
    o = rms_norm(o, norm_g) * jax.nn.silu(g_lin.astype(jnp.float32).reshape(B, S, HG_HEADS, HG_DV))
    return o.reshape(B, S, HG_HEADS * HG_DV).astype(q_lin.dtype)


def fox_branch(q_lin, k_lin, v_lin, f_lin, f_bias):
    B, S, _ = q_lin.shape
    q = q_lin.reshape(B, S, FOX_HEADS, 1, FOX_DH)
    k = k_lin.reshape(B, S, FOX_HEADS, 1, FOX_DH)
    v = v_lin.reshape(B, S, FOX_HEADS, FOX_DH)
    log_f = jax.nn.log_sigmoid(f_lin.astype(jnp.float32) + f_bias.astype(jnp.float32))
    o = causal_block_attention(q, k, v, log_f)
    return o.reshape(B, S, FOX_HEADS * FOX_DH)


def diff_branch(q_lin, k_lin, v_lin, cos, sin, lam_vecs, lam_init, norm_g):
    B, S, _ = q_lin.shape
    q = partial_rope(q_lin.reshape(B, S, DIFF_HEADS, N_DIFF_MAPS, DIFF_DH), cos, sin)
    k = partial_rope(k_lin.reshape(B, S, DIFF_HEADS, N_DIFF_MAPS, DIFF_DH), cos, sin)
    v = v_lin.reshape(B, S, DIFF_HEADS, DIFF_DV)
    o = causal_block_attention(q, k, v).astype(jnp.float32)
    lv = lam_vecs.astype(jnp.float32)
    lam = jnp.exp(jnp.sum(lv[0] * lv[1])) - jnp.exp(jnp.sum(lv[2] * lv[3])) + lam_init
    o = o[:, :, :, 0] - lam * o[:, :, :, 1]
    o = rms_norm(o, norm_g) * (1.0 - lam_init)
    return o.reshape(B, S, DIFF_HEADS * DIFF_DV).astype(q_lin.dtype)


def token_mixer(x, cos, sin, w_in, lb, hg_norm_g, fox_bias, lam_vecs, lam_init, diff_norm_g, w_branch, w_out):
    B, S, D = x.shape
    proj = jnp.einsum("bsd,dn->bsn", x, w_in)
    offsets = [int(o) for o in np.cumsum(IN_SPLITS)[:-1]]
    (hq, hf, hi, hg, fq, fk, fv, ff, dq, dk, dv, gates) = jnp.split(proj, offsets, axis=-1)
    y_a = hgrn2_branch(hq, hf, hi, hg, lb, hg_norm_g)
    y_b = fox_branch(fq, fk, fv, ff, fox_bias)
    y_c = diff_branch(dq, dk, dv, cos, sin, lam_vecs, lam_init, diff_norm_g)
    ys = jnp.stack([y_a, y_b, y_c], axis=2)
    branch = jnp.einsum("bsrw,rwd->bsrd", ys, w_branch)
    g = jax.nn.sigmoid(gates.astype(jnp.float32)).astype(x.dtype).reshape(B, S, N_BRANCHES, D)
    merged = jnp.sum(g * branch, axis=2)
    return jnp.einsum("bsd,de->bse", merged, w_out)


def hier_moe(x, w_rg, b_rg, w_re, b_re, w_gate, w_up, w_down):
    B, S, D = x.shape
    xt = x.reshape(B * S, D)
    g_prob = jax.nn.softmax((xt @ w_rg).astype(jnp.float32) + b_rg.astype(jnp.float32), axis=-1)
    g_p, g_idx = lax.top_k(g_prob, 1)
    e_logits = (xt @ w_re).astype(jnp.float32).reshape(-1, N_GROUPS, EXPERTS_PER_GROUP) + b_re.astype(jnp.float32)
    e_sel = jnp.take_along_axis(e_logits, g_idx[:, :, None], axis=1)[:, 0]
    e_l, e_idx = lax.top_k(e_sel, TOP_K_IN_GROUP)
    w = g_p * jax.nn.softmax(e_l, axis=-1)
    ids = g_idx * EXPERTS_PER_GROUP + e_idx
    gate = jnp.einsum("tk,tke->te", w, jax.nn.one_hot(ids, N_EXPERTS, dtype=jnp.float32)).astype(x.dtype)
    out = jnp.zeros_like(xt)
    for e in range(N_EXPERTS):
        h = jax.nn.silu(xt @ w_gate[e]) * (xt @ w_up[e])
        out = out + gate[:, e:e + 1] * (h @ w_down[e])
    return out.reshape(B, S, D)


def setup_inputs(seed: int = 0) -> dict:
    key = jax.random.key(seed)
    ks = jax.random.split(key, 24)
    f32 = jnp.float32

    def nrm(k, shape, scale):
        return jax.random.normal(k, shape, f32) * scale

    return {
        "x": nrm(ks[0], (BATCH, SEQ, D_MODEL), 1.0),
        "positions": jax.random.randint(ks[1], (BATCH, 1), 0, MAX_POS_OFFSET, dtype=jnp.int32) + jnp.arange(SEQ, dtype=jnp.int32)[None, :],
        "ln_in_g": 1.0 + nrm(ks[2], (D_MODEL,), 0.02),
        "ln_in_b": nrm(ks[3], (D_MODEL,), 0.02),
        "w_in": nrm(ks[4], (DEPTH, D_MODEL, N_IN), D_MODEL ** -0.5),
        "hgrn_lb_logits": nrm(ks[5], (DEPTH, HG_HEADS * HG_DK), 0.1),
        "hgrn_norm_g": 1.0 + nrm(ks[6], (DEPTH, HG_DV), 0.02),
        "fox_f_bias": jax.random.uniform(ks[7], (DEPTH, FOX_HEADS), f32, 2.0, 4.0),
        "diff_lambda": nrm(ks[8], (DEPTH, 4, DIFF_DH), 0.1),
        "diff_norm_g": 1.0 + nrm(ks[9], (DEPTH, DIFF_DV), 0.02),
        "w_branch": nrm(ks[10], (DEPTH, N_BRANCHES, BRANCH_WIDTH, D_MODEL), BRANCH_WIDTH ** -0.5),
        "w_out": nrm(ks[11], (DEPTH, D_MODEL, D_MODEL), D_MODEL ** -0.5 * BETA),
        "ln1_g": 1.0 + nrm(ks[12], (DEPTH, D_MODEL), 0.02),
        "ln1_b": nrm(ks[13], (DEPTH, D_MODEL), 0.02),
        "router_g_w": nrm(ks[14], (DEPTH, D_MODEL, N_GROUPS), D_MODEL ** -0.5),
        "router_g_b": nrm(ks[15], (DEPTH, N_GROUPS), 0.01),
        "router_e_w": nrm(ks[16], (DEPTH, D_MODEL, N_EXPERTS), D_MODEL ** -0.5),
        "router_e_b": nrm(ks[17], (DEPTH, N_GROUPS, EXPERTS_PER_GROUP), 0.01),
        "expert_w_gate": nrm(ks[18], (DEPTH, N_EXPERTS, D_MODEL, D_EXPERT), D_MODEL ** -0.5),
        "expert_w_up": nrm(ks[19], (DEPTH, N_EXPERTS, D_MODEL, D_EXPERT), D_MODEL ** -0.5),
        "expert_w_down": nrm(ks[20], (DEPTH, N_EXPERTS, D_EXPERT, D_MODEL), D_EXPERT ** -0.5 * BETA),
        "ln2_g": 1.0 + nrm(ks[21], (DEPTH, D_MODEL), 0.02),
        "ln2_b": nrm(ks[22], (DEPTH, D_MODEL), 0.02),
    }


def reference(x, positions, ln_in_g, ln_in_b, w_in, hgrn_lb_logits, hgrn_norm_g, fox_f_bias,
              diff_lambda, diff_norm_g, w_branch, w_out, ln1_g, ln1_b, router_g_w, router_g_b,
              router_e_w, router_e_b, expert_w_gate, expert_w_up, expert_w_down, ln2_g, ln2_b):
    inv_freq = ROPE_THETA ** (-jnp.arange(0, ROPE_DIM, 2, dtype=jnp.float32) / ROPE_DIM)
    ang = positions.astype(jnp.float32)[..., None] * inv_freq
    cos, sin = jnp.cos(ang), jnp.sin(ang)
    lb_soft = jax.nn.softmax(hgrn_lb_logits.astype(jnp.float32), axis=0)
    lower_bounds = jnp.maximum(jnp.cumsum(lb_soft, axis=0) - lb_soft[0], 0.0)

    x = layer_norm(x, ln_in_g, ln_in_b)
    for l in range(DEPTH):
        lam_init = 0.8 - 0.6 * float(np.exp(-0.3 * l))
        h = token_mixer(x, cos, sin, w_in[l], lower_bounds[l], hgrn_norm_g[l], fox_f_bias[l],
                        diff_lambda[l], lam_init, diff_norm_g[l], w_branch[l], w_out[l])
        x = layer_norm(ALPHA * x + h, ln1_g[l], ln1_b[l])
        h = hier_moe(x, router_g_w[l], router_g_b[l], router_e_w[l], router_e_b[l],
                     expert_w_gate[l], expert_w_up[l], expert_w_down[l])
        x = layer_norm(ALPHA * x + h, ln2_g[l], ln2_b[l])
    return x
```

```python
import math
from contextlib import ExitStack
import numpy as np
import concourse.bass as bass
import concourse.mybir as mybir
from concourse.bass_utils import run_bass_kernel_spmd

F32 = mybir.dt.float32
BF16 = mybir.dt.bfloat16
I32 = mybir.dt.int32
AF = mybir.ActivationFunctionType
ALU = mybir.AluOpType
AX = mybir.AxisListType

D = 1024
KC = 8
NIN = 8200
DEPTH = 2
NEXP = 32
DEXP = 512
ALPHA = (2 * DEPTH) ** 0.25
LN_EPS = 1e-5
RMS_EPS = 1e-6
O_HQ, O_HF, O_HI, O_HG = 0, 512, 1024, 1536
O_FQ, O_FK, O_FV, O_FF = 2048, 2560, 3072, 3584
O_DQ, O_DK, O_DV, O_GT = 3592, 4104, 4616, 5128
NDMASEM = 8


class Sched:
    ENGS = ("pe", "act", "dve", "pool", "sp")

    def __init__(self):
        self.q = {e: [] for e in self.ENGS}
        self.cnt = {e: 0 for e in self.ENGS}
        self.dcnt = {"sp": 0, "pool": 0}
        self.known = {e: {} for e in self.ENGS}
        self.w = {}
        self.r = {}
        self.all_tokens = {}

    def _deps(self, eng, reads, writes):
        deps = {}

        def add(tok):
            if tok is None:
                return
            k, v = tok
            if eng == "pe" and k == "pe":
                return
            if deps.get(k, 0) < v:
                deps[k] = v

        for r in reads:
            add(self.w.get(r))
        for r in writes:
            add(self.w.get(r))
            for tok in self.r.get(r, {}).items():
                add(tok)
        return deps

    def _commit(self, tok, reads, writes):
        for r in reads:
            d = self.r.setdefault(r, {})
            if d.get(tok[0], 0) < tok[1]:
                d[tok[0]] = tok[1]
        for r in writes:
            self.w[r] = tok
            self.r[r] = {}
        self.all_tokens[tok[0]] = max(self.all_tokens.get(tok[0], 0), tok[1])

    def _waits(self, eng, deps):
        waits = []
        kn = self.known[eng]
        for k, v in deps.items():
            if kn.get(k, 0) < v:
                waits.append((k, v))
                kn[k] = v
        return waits

    def op(self, eng, fn, r=(), w=()):
        deps = self._deps(eng, r, w)
        waits = self._waits(eng, deps)
        self.cnt[eng] += 1
        tok = (eng, self.cnt[eng])
        self.q[eng].append((waits, fn, (eng, 1)))
        self._commit(tok, r, w)

    def dma(self, qeng, fn, r=(), w=()):
        deps = self._deps(qeng, r, w)
        n = self.dcnt[qeng]
        self.dcnt[qeng] += 1
        key = ("dma", qeng, n % NDMASEM)
        val = 16 * (n // NDMASEM + 1)
        if val > 16:
            deps[key] = max(deps.get(key, 0), val - 16)
        waits = self._waits(qeng, deps)
        self.q[qeng].append((waits, fn, (key, 16)))
        self._commit((key, val), r, w)

    def barrier(self):
        for e in self.ENGS:
            waits = self._waits(e, dict(self.all_tokens))
            if waits:
                self.q[e].append((waits, None, None))

    def emit(self, nc, es):
        sems = {}
        for e in self.ENGS:
            sems[e] = es.enter_context(nc.semaphore("s_" + e))
        for qe in ("sp", "pool"):
            for i in range(NDMASEM):
                sems[("dma", qe, i)] = es.enter_context(nc.semaphore("d_%s%d" % (qe, i)))
        block = es.enter_context(nc.Block())

        def run(ename, eobj):
            for waits, fn, inc in self.q[ename]:
                for k, v in waits:
                    eobj.wait_ge(sems[k], v)
                if fn is not None:
                    ins = fn(eobj)
                    ins.then_inc(sems[inc[0]], inc[1])

        @block.tensor
        def _(e):
            run("pe", e)

        @block.scalar
        def _(e):
            run("act", e)

        @block.vector
        def _(e):
            run("dve", e)

        @block.gpsimd
        def _(e):
            run("pool", e)
            for k, v in self.all_tokens.items():
                e.wait_ge(sems[k], v)

        @block.sync
        def _(e):
            run("sp", e)
            for k, v in self.all_tokens.items():
                e.wait_ge(sems[k], v)


class Arena:
    def __init__(self, ap, nwords):
        self.ap = ap
        self.n = nwords
        self.top = 0
        self.uid = 0

    def mark(self):
        return self.top

    def release(self, m):
        self.top = m

    def f32(self, n, name):
        a = self.top
        self.top += n
        assert self.top <= self.n, "SBUF arena overflow at %s: %d > %d" % (name, self.top, self.n)
        return self.ap[:, a:a + n]

    def bf16(self, n, name):
        nw = (n + 1) // 2
        return self.f32(nw, name).bitcast(BF16)[:, 0:n]


def host_consts():
    c = {}
    c["ident"] = np.eye(128, dtype=np.float32)
    s = np.arange(128)[:, None]
    t = np.arange(128)[None, :]
    c["tri"] = (s <= t).astype(np.float32)
    c["trih"] = ((s <= t) & ((s // 64) == (t // 64))).astype(np.float32)
    c["ones"] = np.ones((128, 128), np.float32)
    rm = np.ones((128, 512), np.float32)
    rm[:, 0::64] = 0.0
    c["rmask"] = rm
    es = np.zeros((128, 8, 128), np.float32)
    for h in range(8):
        for b in (0, 32, 64):
            es[b + h, h, :] = 1.0
    c["esel"] = es.reshape(128, 1024)
    p = np.arange(128)
    d = p % 64
    invf = np.where(d < 16, 500000.0 ** (-(2.0 * (d % 8)) / 16.0), 0.0)
    sgn = np.where(d < 8, -1.0, np.where(d < 16, 1.0, 0.0))
    misc = np.zeros((128, 8), np.float32)
    misc[:, 0] = invf / (2.0 * math.pi)
    misc[:, 1] = sgn
    c["misc"] = misc
    order = ["ident", "tri", "trih", "ones", "rmask", "esel", "misc"]
    offs = {}
    o = 0
    for k in order:
        offs[k] = (o, c[k].shape[1])
        o += c[k].shape[1]
    arr = np.concatenate([c[k] for k in order], axis=1).astype(np.float32)
    return arr, offs


CONST_ARR, CONST_OFF = host_consts()
NCONST = CONST_ARR.shape[1]


def build_program(S, NSEQ, L=DEPTH, dbg=None):
    global LAST_SCHED
    NT = S // 128
    NB = S // 512
    nc = bass.Bass("TRN2", target_bir_lowering=False)
    dram = {}

    def din(name, shape, dt=F32):
        dram[name] = nc.dram_tensor(name, list(shape), dt, kind="ExternalInput").ap()
        return dram[name]

    x_in = din("x", [NSEQ * S, D])
    pos_in = din("pos", [NSEQ, S], I32)
    consts_in = din("consts", [128, NCONST])
    ln_in_g = din("ln_in_g", [D]); ln_in_b = din("ln_in_b", [D])
    w_in = din("w_in", [L, D, NIN])
    w_sw = din("w_sw", [L, D, 1024])
    lb_logits = din("hgrn_lb_logits", [L, 512])
    hg_norm = din("hgrn_norm_g", [L, 128])
    fox_bias = din("fox_f_bias", [L, 8])
    dlam = din("diff_lambda", [L, 4, 64])
    dnorm = din("diff_norm_g", [L, 128])
    w_branch = din("w_branch", [L, 3, 512, D])
    w_out = din("w_out", [L, D, D])
    ln1_g = din("ln1_g", [L, D]); ln1_b = din("ln1_b", [L, D])
    rg_w = din("router_g_w", [L, D, 4]); rg_b = din("router_g_b", [L, 4])
    re_w = din("router_e_w", [L, D, 32]); re_b = din("router_e_b", [L, 32])
    ew_g = din("expert_w_gate", [L, NEXP, D, DEXP])
    ew_u = din("expert_w_up", [L, NEXP, D, DEXP])
    ew_d = din("expert_w_down", [L, NEXP, DEXP, D])
    ln2_g = din("ln2_g", [L, D]); ln2_b = din("ln2_b", [L, D])
    y_out = nc.dram_tensor("y", [NSEQ * S, D], F32, kind="ExternalOutput").ap()
    xres = nc.dram_tensor("xres_scratch", [S, D], F32).ap()
    dbg_out = None
    if dbg is not None:
        dbg_out = nc.dram_tensor("dbg", list(dbg[1]), F32, kind="ExternalOutput").ap()

    sc = Sched()
    LAST_SCHED = sc
    es = ExitStack()
    NW = 52992
    arena_t = es.enter_context(nc.sbuf_tensor("arena", [128, NW], F32))
    ar = Arena(arena_t, NW)
    PS = [es.enter_context(nc.psum_tensor("ps%d" % i, [128, 512], F32)) for i in range(8)]

    def psk(i):
        return ("ps", i)

    CF = ar.f32(NCONST, "consts")
    sc.dma("sp", lambda e: e.dma_start(out=CF, in_=consts_in[:, :]), w=["CF"])

    def cf(name):
        o, n = CONST_OFF[name]
        return CF[:, o:o + n]

    IDF = cf("ident")
    ONESF = cf("ones")
    RMASK = cf("rmask")
    MISC = cf("misc")
    CB = ar.bf16(128 * 4 + 1024, "constbf")
    IDB = CB[:, 0:128]; TRIB = CB[:, 128:256]; TRIHB = CB[:, 256:384]; ONESB = CB[:, 384:512]
    ESELB = CB[:, 512:1536]
    sc.op("dve", lambda e: e.tensor_copy(out=CB[:, 0:512], in_=CF[:, 0:512]), r=["CF"], w=["CB"])
    o_es = CONST_OFF["esel"][0]
    sc.op("dve", lambda e: e.tensor_copy(out=ESELB, in_=CF[:, o_es:o_es + 1024]), r=["CF"], w=["CB"])

    SM = ar.f32(64, "small")
    smc = [0]

    def small(n):
        a = smc[0]
        smc[0] += n
        assert smc[0] <= 64
        return SM[:, a:a + n]

    LBL = small(L * 4)
    LB = small(L * 4)
    OML = small(L * 4)
    NOML = small(L * 4)
    HGN = small(L)
    DNG = small(L)
    NLAM = small(L)
    FB = small(L)
    TMP4 = small(8)
    sc.dma("sp", lambda e: e.dma_start(out=LBL.rearrange("p (l h) -> p l h", l=L),
                                       in_=lb_logits.rearrange("l (h c) -> c l h", c=128), allow_slow_non_contiguous=True), w=["SM"])
    sc.dma("sp", lambda e: e.dma_start(out=HGN, in_=hg_norm.rearrange("l v -> v l"), allow_slow_non_contiguous=True), w=["SM"])
    sc.dma("sp", lambda e: e.dma_start(out=DNG, in_=dnorm.rearrange("l v -> v l"), allow_slow_non_contiguous=True), w=["SM"])
    sc.op("dve", lambda e: e.memset(FB, 0.0), w=["SM"])
    for b0 in (0, 32, 64):
        sc.dma("sp", lambda e, b0=b0: e.dma_start(out=FB[b0:b0 + 8, :], in_=fox_bias.rearrange("l h -> h l"), allow_slow_non_contiguous=True),
               r=["SM"], w=["SM"])
    sc.op("dve", lambda e: e.tensor_scalar(out=FB, in0=FB, scalar1=-1.0, scalar2=None, op0=ALU.mult), r=["SM"], w=["SM"])
    EXL = small(L * 4)
    sc.op("act", lambda e: e.activation(out=EXL, in_=LBL, func=AF.Exp), r=["SM"], w=["SM"])
    SUM4 = TMP4[:, 0:4]
    sc.op("dve", lambda e: e.tensor_tensor(out=SUM4, in0=EXL[:, 0:4], in1=EXL[:, 4:8], op=ALU.add), r=["SM"], w=["SM"])
    sc.op("dve", lambda e: e.reciprocal(out=SUM4, in_=SUM4), r=["SM"], w=["SM"])
    SMX = small(L * 4)
    for l in range(L):
        sc.op("dve", lambda e, l=l: e.tensor_tensor(out=SMX[:, l * 4:l * 4 + 4], in0=EXL[:, l * 4:l * 4 + 4], in1=SUM4, op=ALU.mult),
              r=["SM"], w=["SM"])
    sc.op("dve", lambda e: e.tensor_tensor(out=LB[:, 0:4], in0=SMX[:, 0:4], in1=SMX[:, 0:4], op=ALU.subtract), r=["SM"], w=["SM"])
    sc.op("dve", lambda e: e.tensor_tensor(out=LB[:, 4:8], in0=SMX[:, 0:4], in1=SMX[:, 4:8], op=ALU.add), r=["SM"], w=["SM"])
    sc.op("dve", lambda e: e.tensor_tensor(out=LB[:, 4:8], in0=LB[:, 4:8], in1=SMX[:, 0:4], op=ALU.subtract), r=["SM"], w=["SM"])
    sc.op("dve", lambda e: e.tensor_scalar(out=LB, in0=LB, scalar1=0.0, scalar2=None, op0=ALU.max), r=["SM"], w=["SM"])
    sc.op("dve", lambda e: e.tensor_scalar(out=OML, in0=LB, scalar1=-1.0, scalar2=1.0, op0=ALU.mult, op1=ALU.add), r=["SM"], w=["SM"])
    sc.op("dve", lambda e: e.tensor_scalar(out=NOML, in0=OML, scalar1=-1.0, scalar2=None, op0=ALU.mult), r=["SM"], w=["SM"])
    LV = ar.f32(L * 256, "lamvec")
    sc.dma("sp", lambda e: e.dma_start(out=LV, in_=dlam.rearrange("l a d -> (l a d)").partition_broadcast(128)), w=["LV"])
    for l in range(L):
        lam_init = 0.8 - 0.6 * float(np.exp(-0.3 * l))
        base = l * 256
        P1 = LV[:, base:base + 64]
        P2 = LV[:, base + 128:base + 192]
        sc.op("dve", lambda e, P1=P1, base=base: e.tensor_tensor(out=P1, in0=P1, in1=LV[:, base + 64:base + 128], op=ALU.mult), r=["LV"], w=["LV"])
        sc.op("dve", lambda e, P2=P2, base=base: e.tensor_tensor(out=P2, in0=P2, in1=LV[:, base + 192:base + 256], op=ALU.mult), r=["LV"], w=["LV"])
        sc.op("dve", lambda e, P1=P1: e.tensor_reduce(out=TMP4[:, 4:5], in_=P1, axis=AX.X, op=ALU.add), r=["LV", "SM"], w=["SM"])
        sc.op("dve", lambda e, P2=P2: e.tensor_reduce(out=TMP4[:, 5:6], in_=P2, axis=AX.X, op=ALU.add), r=["LV", "SM"], w=["SM"])
        sc.op("act", lambda e: e.activation(out=TMP4[:, 4:6], in_=TMP4[:, 4:6], func=AF.Exp), r=["SM"], w=["SM"])
        sc.op("dve", lambda e: e.tensor_tensor(out=TMP4[:, 6:7], in0=TMP4[:, 5:6], in1=TMP4[:, 4:5], op=ALU.subtract), r=["SM"], w=["SM"])
        sc.op("dve", lambda e, l=l, lam_init=lam_init: e.tensor_scalar(out=NLAM[:, l:l + 1], in0=TMP4[:, 6:7], scalar1=-lam_init, scalar2=None, op0=ALU.add), r=["SM"], w=["SM"])
        sc.op("dve", lambda e, l=l, lam_init=lam_init: e.tensor_scalar(out=DNG[:, l:l + 1], in0=DNG[:, l:l + 1], scalar1=1.0 - lam_init, scalar2=None, op0=ALU.mult), r=["SM"], w=["SM"])

    XT = ar.bf16(KC * S, "XT").rearrange("p (k t) -> p k t", k=KC)
    xt_lo = ar.top - KC * S // 2
    MT = ar.bf16(KC * S, "MT").rearrange("p (k t) -> p k t", k=KC)
    mt_lo = ar.top - KC * S // 2
    ACCF = ar.f32(NT * D, "ACC")
    acc_lo = ar.top - NT * D
    ACC = ACCF.rearrange("p (t d) -> p t d", t=NT)
    GATE = ar.f32(NT * NEXP, "GATE").rearrange("p (t e) -> p t e", t=NT)
    LNG = ar.f32(D, "LNG"); LNB = ar.f32(D, "LNB")
    CCOL = ar.f32(8, "ccol")
    tail_lo = ar.top
    A_XT = Arena(arena_t[:, xt_lo:xt_lo + KC * S // 2], KC * S // 2)
    A_MT = Arena(arena_t[:, mt_lo:mt_lo + KC * S // 2], KC * S // 2)
    A_ACC = Arena(arena_t[:, acc_lo:acc_lo + NT * D], NT * D)
    A_TL = Arena(arena_t[:, tail_lo:NW], NW - tail_lo)
    YT = A_ACC.bf16(12 * S, "YT").rearrange("p (c t) -> p c t", c=12)
    yt_mark = A_ACC.mark()

    ccols = {}

    def constcol(v):
        if v not in ccols:
            i = len(ccols)
            assert i < 8
            ccols[v] = CCOL[:, i:i + 1]
            sc.op("dve", lambda e, i=i, v=v: e.memset(CCOL[:, i:i + 1], float(v)), w=[("ccol", i)])
        return ccols[v]

    C_EPS_RMS = constcol(RMS_EPS)
    C_EPS_LN = constcol(LN_EPS)
    C_ONE = constcol(1.0)
    SINSC = 2.0 * math.pi * (1.0 - 1e-6)
    C_SINB = constcol(-math.pi * (1.0 - 1e-6))
    CK = [("ccol", i) for i in range(8)]

    def load_ln(g_ap, b_ap):
        sc.dma("sp", lambda e: e.dma_start(out=LNG, in_=g_ap.partition_broadcast(128)), w=["LNG"])
        sc.dma("sp", lambda e: e.dma_start(out=LNB, in_=b_ap.partition_broadcast(128)), w=["LNB"])

    def wview(l, c0, c1):
        return w_in[l].rearrange("(k p) n -> p k n", p=128)[:, :, c0:c1]

    def wtile(arena, ncols, name):
        return arena.bf16(KC * ncols, name).rearrange("p (k n) -> p k n", k=KC)

    def load_w(dst3, src3, key):
        sc.dma("pool", lambda e: e.dma_start(out=dst3, in_=src3), w=[key])

    def xkeys(xkey, blk):
        return [(xkey, blk * 4 + i) for i in range(4)]

    def proj_fm(bank, wt3, ncols, blk, wkey, xt=None, xkey="XT"):
        xt = XT if xt is None else xt
        for kc in range(KC):
            sc.op("pe", lambda e, kc=kc: e.matmul(PS[bank][0:ncols, :], lhsT=wt3[:, kc, 0:ncols],
                                                  rhs=xt[:, kc, blk * 512:(blk + 1) * 512],
                                                  start=(kc == 0), stop=(kc == KC - 1)),
                  r=[wkey] + xkeys(xkey, blk), w=[psk(bank)])

    def proj_tm(bank, c0, wt3, ncols, t, wkey, xt=None, xkey="XT"):
        xt = XT if xt is None else xt
        for kc in range(KC):
            sc.op("pe", lambda e, kc=kc: e.matmul(PS[bank][:, c0:c0 + ncols], lhsT=xt[:, kc, t * 128:(t + 1) * 128],
                                                  rhs=wt3[:, kc, 0:ncols],
                                                  start=(kc == 0), stop=(kc == KC - 1)),
                  r=[wkey, (xkey, t)], w=[psk(bank)])

    def ln_and_xt(tiles, SMALL, XB, dst, dkey, post=None):
        def sm(i):
            return SMALL[:, i * 16:(i + 1) * 16]
        for i, (t, xa, xk) in enumerate(tiles):
            for hf in range(2):
                sc.op("dve", lambda e, i=i, xa=xa, hf=hf: e.bn_stats(out=sm(i)[:, hf * 6:(hf + 1) * 6], in_=xa[:, hf * 512:(hf + 1) * 512]),
                      r=[xk], w=[("sm", i)])
        for i, (t, xa, xk) in enumerate(tiles):
            sc.op("dve", lambda e, i=i: e.bn_aggr(out=sm(i)[:, 12:14], in_=sm(i)[:, 0:12]), r=[("sm", i)], w=[("sm", i)])
        for i, (t, xa, xk) in enumerate(tiles):
            sc.op("act", lambda e, i=i: e.activation(out=sm(i)[:, 14:15], in_=sm(i)[:, 13:14], func=AF.Sqrt, bias=C_EPS_LN, scale=1.0),
                  r=[("sm", i)] + CK, w=[("sm", i)])
        for i, (t, xa, xk) in enumerate(tiles):
            sc.op("dve", lambda e, i=i: e.reciprocal(out=sm(i)[:, 14:15], in_=sm(i)[:, 14:15]), r=[("sm", i)], w=[("sm", i)])

        def evac(i):
            t, xa, xk = tiles[i]
            bank = 6 + (i % 2)
            pb = PS[bank].bitcast(BF16)
            sc.op("dve", lambda e: e.tensor_copy(out=dst[:, :, t * 128:(t + 1) * 128], in_=pb.rearrange("p (k c) -> p k c", k=KC)),
                  r=[psk(bank)], w=[(dkey, t)])

        for i, (t, xa, xk) in enumerate(tiles):
            sc.op("dve", lambda e, i=i, xa=xa: e.scalar_tensor_tensor(out=xa, in0=xa, scalar=sm(i)[:, 12:13], in1=LNG, op0=ALU.subtract, op1=ALU.mult),
                  r=[xk, ("sm", i), "LNG"], w=[xk])
            sc.op("dve", lambda e, i=i, xa=xa: e.scalar_tensor_tensor(out=xa, in0=xa, scalar=sm(i)[:, 14:15], in1=LNB, op0=ALU.mult, op1=ALU.add),
                  r=[xk, ("sm", i), "LNB"], w=[xk])
            if post is not None:
                post(t, xa, xk)
            if dst is not None:
                xb = XB[i % 2]
                bank = 6 + (i % 2)
                pb = PS[bank].bitcast(BF16)
                sc.op("act", lambda e, xa=xa, xb=xb: e.activation(out=xb, in_=xa, func=AF.Copy), r=[xk], w=[("xb", i % 2)])
                for kc in range(KC):
                    sc.op("pe", lambda e, kc=kc, xb=xb, pb=pb: e.transpose(out=pb[:, kc * 128:(kc + 1) * 128], in_=xb[:, kc * 128:(kc + 1) * 128], identity=IDB),
                          r=[("xb", i % 2), "CB"], w=[psk(bank)])
                if i >= 1:
                    evac(i - 1)
        if dst is not None:
            evac(len(tiles) - 1)

    def rms_finish(src, srckey, sq, sqkey, rs, rskey, bank):
        sc.op("act", lambda e: e.activation(out=sq, in_=src, func=AF.Square), r=[srckey], w=[sqkey])
        sc.op("pe", lambda e: e.matmul(PS[bank][:, :], lhsT=ONESF, rhs=sq, start=True, stop=True), r=[sqkey, "CF"], w=[psk(bank)])
        sc.op("act", lambda e: e.activation(out=rs, in_=PS[bank][:, :], func=AF.Sqrt, bias=C_EPS_RMS, scale=1.0 / 128.0),
              r=[psk(bank)] + CK, w=[rskey])
        sc.op("dve", lambda e: e.reciprocal(out=rs, in_=rs), r=[rskey], w=[rskey])

    def hgrn_walloc(wk, par):
        return [wtile(wk, 128, "hw%d_%d" % (par, i)) for i in range(4)] + [("hw", par)]

    def hgrn_load(l, h, W):
        for i, o in enumerate((O_HQ, O_HF, O_HI, O_HG)):
            load_w(W[i], wview(l, o + h * 128, o + (h + 1) * 128), W[4])

    def hgrn_unit(l, h, wk, W):
        mk = wk.mark()
        li = l * 4 + h
        WQ, WF, WI, WG, wkey_ = W
        VT = wk.bf16(NT * 128, "vt").rearrange("p (t v) -> p t v", t=NT)
        ST = wk.f32(128, "st")
        KDT = [wk.bf16(128, "kdt%d" % i) for i in range(2)]
        AT = [wk.bf16(128, "at%d" % i) for i in range(2)]
        SG = wk.f32(512, "sg"); QS = wk.f32(512, "qs"); BB = wk.f32(512, "bb"); ENB = wk.f32(512, "enb")
        GSs = [wk.f32(512, "gs%d" % i) for i in range(2)]; SQs = [wk.f32(512, "sq%d" % i) for i in range(2)]
        RSs = [wk.f32(512, "rs%d" % i) for i in range(2)]; EBs = [wk.f32(512, "eb%d" % i) for i in range(2)]
        QTLs = [wk.f32(512, "qtl%d" % i) for i in range(2)]; KTLs = [wk.f32(512, "ktl%d" % i) for i in range(2)]
        KDLs = [wk.bf16(512, "kdl%d" % i) for i in range(2)]
        sc.op("dve", lambda e: e.memset(ST, 0.0), w=["ST"])
        pb4 = PS[5].bitcast(BF16)
        for blk in range(NB):
            bp = blk % 2
            GS = GSs[bp]; SQ = SQs[bp]; RS = RSs[bp]; EB = EBs[bp]; QTL = QTLs[bp]; KTL = KTLs[bp]; KDL = KDLs[bp]
            kGS = ("GS", bp); kSQ = ("SQ", bp); kRS = ("RS", bp); kEB = ("EB", bp); kQ = ("QTL", bp); kK = ("KTL", bp); kKD = ("KDL", bp)
            ob = 6 + bp
            bc = slice(blk * 512, (blk + 1) * 512)
            for tt in range(4):
                proj_tm(3, tt * 128, WI, 128, blk * 4 + tt, wkey_)
            sc.op("act", lambda e, blk=blk: e.activation(out=VT[:, blk * 4:(blk + 1) * 4, :], in_=PS[3][:, :].rearrange("p (t v) -> p t v", t=4), func=AF.Copy),
                  r=[psk(3)], w=["VT"])
            proj_fm(0, WQ, 128, blk, wkey_)
            proj_fm(1, WF, 128, blk, wkey_)
            proj_fm(2, WG, 128, blk, wkey_)
            sc.op("act", lambda e: e.activation(out=SG, in_=PS[1][:, :], func=AF.Sigmoid), r=[psk(1)], w=["SG"])
            sc.op("act", lambda e: e.activation(out=QS, in_=PS[0][:, :], func=AF.Silu), r=[psk(0)], w=["QS"])
            sc.op("act", lambda e, GS=GS: e.activation(out=GS, in_=PS[2][:, :], func=AF.Silu), r=[psk(2)], w=[kGS])
            sc.op("dve", lambda e, EB=EB: e.tensor_scalar(out=EB, in0=SG, scalar1=OML[:, li:li + 1], scalar2=LB[:, li:li + 1], op0=ALU.mult, op1=ALU.add),
                  r=["SG", "SM"], w=[kEB])
            sc.op("act", lambda e, EB=EB: e.activation(out=EB, in_=EB, func=AF.Ln), r=[kEB], w=[kEB])
            sc.op("dve", lambda e, EB=EB: e.tensor_tensor_scan(out=BB, data0=RMASK, data1=EB, initial=0.0, op0=ALU.mult, op1=ALU.add),
                  r=[kEB, "CF"], w=["BB"])
            sc.op("act", lambda e, EB=EB: e.activation(out=EB, in_=BB, func=AF.Exp), r=["BB"], w=[kEB])
            sc.op("act", lambda e: e.activation(out=ENB, in_=BB, func=AF.Exp, scale=-1.0), r=["BB"], w=["ENB"])
            sc.op("dve", lambda e, EB=EB, QTL=QTL: e.tensor_tensor(out=QTL, in0=QS, in1=EB, op=ALU.mult), r=["QS", kEB], w=[kQ])
            sc.op("dve", lambda e: e.tensor_scalar(out=SG, in0=SG, scalar1=NOML[:, li:li + 1], scalar2=OML[:, li:li + 1], op0=ALU.mult, op1=ALU.add),
                  r=["SG", "SM"], w=["SG"])
            sc.op("dve", lambda e, KTL=KTL: e.tensor_tensor(out=KTL, in0=SG, in1=ENB, op=ALU.mult), r=["SG", "ENB"], w=[kK])
            for ch in range(8):
                cs = slice(ch * 64, (ch + 1) * 64)
                sc.op("dve", lambda e, cs=cs, ch=ch, KDL=KDL, KTL=KTL, EB=EB: e.tensor_scalar(out=KDL[:, cs], in0=KTL[:, cs], scalar1=EB[:, ch * 64 + 63:ch * 64 + 64], scalar2=None, op0=ALU.mult),
                      r=[kK, kEB], w=[kKD])
            for tt in range(4):
                T = blk * 4 + tt
                c0 = tt * 128
                p2 = T % 2
                tcol = slice(p2 * 128, (p2 + 1) * 128)
                scol = slice(p2 * 128, (p2 + 1) * 128)
                sc.op("pe", lambda e, c0=c0, tcol=tcol, KDL=KDL: e.transpose(out=pb4[:, tcol], in_=KDL[:, c0:c0 + 128], identity=IDB),
                      r=[kKD, "CB"], w=[("ps5t", p2)])
                sc.op("act", lambda e, p2=p2, tcol=tcol: e.activation(out=KDT[p2], in_=pb4[:, tcol], func=AF.Copy), r=[("ps5t", p2)], w=[("KDT", p2)])
                sc.op("pe", lambda e, c0=c0, scol=scol, KTL=KTL, QTL=QTL: e.matmul(PS[4][:, scol], lhsT=KTL[:, c0:c0 + 128], rhs=QTL[:, c0:c0 + 128], start=True, stop=True),
                      r=[kK, kQ], w=[("ps4s", p2)])
                sc.op("dve", lambda e, p2=p2, scol=scol: e.tensor_tensor(out=AT[p2], in0=PS[4][:, scol], in1=TRIHB, op=ALU.mult), r=[("ps4s", p2), "CB"], w=[("AT", p2)])
                sc.op("pe", lambda e, c0=c0, p2=p2, T=T, ob=ob: e.matmul(PS[ob][:, c0:c0 + 128], lhsT=VT[:, T, :], rhs=AT[p2], start=True, stop=False),
                      r=["VT", ("AT", p2)], w=[psk(ob)])
                for ch in range(2):
                    cc = c0 + ch * 64
                    rows = slice(ch * 64, (ch + 1) * 64)
                    sc.op("pe", lambda e, cc=cc, ch=ch, ob=ob, QTL=QTL: e.matmul(PS[ob][:, cc:cc + 64], lhsT=ST, rhs=QTL[:, cc:cc + 64], start=False, stop=(ch == 1)),
                          r=["ST", kQ], w=[psk(ob)])
                    sc.op("pe", lambda e, rows=rows, p2=p2, T=T: e.matmul(PS[3][:, 0:128], lhsT=KDT[p2][rows, :], rhs=VT[rows, T, :], start=True, stop=True),
                          r=[("KDT", p2), "VT"], w=[psk(3)])
                    sc.op("dve", lambda e, cc=cc, EB=EB: e.scalar_tensor_tensor(out=ST, in0=ST, scalar=EB[:, cc + 63:cc + 64], in1=PS[3][:, 0:128], op0=ALU.mult, op1=ALU.add),
                          r=["ST", kEB, psk(3)], w=["ST"])
            rms_finish(PS[ob][:, :], psk(ob), SQ, kSQ, RS, kRS, 2)
            sc.op("dve", lambda e, ob=ob, SQ=SQ, RS=RS: e.tensor_tensor(out=SQ, in0=PS[ob][:, :], in1=RS, op=ALU.mult), r=[psk(ob), kRS, kSQ], w=[kSQ])
            sc.op("dve", lambda e, bc=bc, SQ=SQ, GS=GS: e.scalar_tensor_tensor(out=YT[:, h, bc], in0=SQ, scalar=HGN[:, l:l + 1], in1=GS, op0=ALU.mult, op1=ALU.mult),
                  r=[kSQ, kGS, "SM"], w=[("YT", h)])
        wk.release(mk)

    def attn_walloc(wk, par, n):
        return [wtile(wk, 128, "aw%d_%d" % (par, i)) for i in range(n)] + [("aw", par)]

    def attn_load(l, kind, u, W):
        if kind == "fox":
            offs = (O_FQ, O_FK, O_FV)
        else:
            offs = (O_DQ, O_DK, O_DV)
        for i, o in enumerate(offs):
            load_w(W[i], wview(l, o + u * 128, o + (u + 1) * 128), W[-1])
        if kind != "fox":
            swv = w_sw[l].rearrange("(k p) n -> p k n", p=128)
            load_w(W[3], swv[:, :, u * 128:(u + 1) * 128], W[-1])
            load_w(W[4], swv[:, :, 512 + u * 128:512 + (u + 1) * 128], W[-1])

    def attn_unit(l, kind, u, wk, W, fox=None, rope=None):
        mk = wk.mark()
        is_fox = kind == "fox"
        WQ, WK, WV = W[0], W[1], W[2]
        wkey_ = W[-1]
        if not is_fox:
            WQS, WKS = W[3], W[4]
            T1s = [wk.f32(512, "t1_%d" % i) for i in range(2)]; T2s = [wk.f32(512, "t2_%d" % i) for i in range(2)]
            O0 = wk.f32(512, "o0"); OD = wk.f32(512, "od"); SQ = wk.f32(512, "asq"); RS = wk.f32(512, "ars")
            CT, STB_ = rope
        QT = wk.bf16(S, "qt"); KT = wk.bf16(S, "kt")
        if is_fox:
            VT2 = wk.bf16(NT * 256, "avt2").rearrange("p (t m v) -> p t m v", t=NT, m=2)
            sc.op("dve", lambda e: e.memset(VT2[:, :, 0, 64:128], 1.0), w=["AVT"])
            sc.op("dve", lambda e: e.memset(VT2[:, :, 1, 0:64], 1.0), w=["AVT"])
        else:
            VT = wk.bf16(NT * 128, "avt").rearrange("p (t v) -> p t v", t=NT)
        PT = [wk.bf16(512, "pt%d" % i) for i in range(3)]
        SBK = (0, 1, 7)
        RD = wk.f32(512, "rd")
        for blk in range(NB):
            bc = slice(blk * 512, (blk + 1) * 512)
            for tt in range(4):
                proj_tm(6, tt * 128, WV, 128, blk * 4 + tt, wkey_)
            pv6 = PS[6][:, :].rearrange("p (t v) -> p t v", t=4)
            if is_fox:
                sc.op("act", lambda e, blk=blk, pv6=pv6: e.activation(out=VT2[:, blk * 4:(blk + 1) * 4, 0, 0:64], in_=pv6[:, :, 0:64], func=AF.Copy), r=[psk(6)], w=["AVT"])
                sc.op("act", lambda e, blk=blk, pv6=pv6: e.activation(out=VT2[:, blk * 4:(blk + 1) * 4, 1, 64:128], in_=pv6[:, :, 64:128], func=AF.Copy), r=[psk(6)], w=["AVT"])
            else:
                sc.op("act", lambda e, blk=blk, pv6=pv6: e.activation(out=VT[:, blk * 4:(blk + 1) * 4, :], in_=pv6, func=AF.Copy), r=[psk(6)], w=["AVT"])
            proj_fm(4, WQ, 128, blk, wkey_)
            proj_fm(5, WK, 128, blk, wkey_)
            if is_fox:
                sc.op("act", lambda e, bc=bc: e.activation(out=QT[:, bc], in_=PS[4][:, :], func=AF.Copy, scale=0.125), r=[psk(4)], w=["QT"])
                sc.op("dve", lambda e, bc=bc: e.tensor_copy(out=KT[:, bc], in_=PS[5][:, :]), r=[psk(5)], w=["KT"])
            else:
                for (ii, bank, WS, wkey, dst, dkey) in ((0, 4, WQS, wkey_, QT, "QT"), (1, 5, WKS, wkey_, KT, "KT")):
                    T1 = T1s[ii]; T2 = T2s[ii]
                    sc.op("dve", lambda e, bank=bank, bc=bc, T1=T1: e.tensor_tensor(out=T1, in0=PS[bank][:, :], in1=CT[:, bc], op=ALU.mult), r=[psk(bank), "ROPE"], w=[("T1", ii)])
                    proj_fm(bank, WS, 128, blk, wkey)
                    sc.op("dve", lambda e, bank=bank, bc=bc, T2=T2: e.tensor_tensor(out=T2, in0=PS[bank][:, :], in1=STB_[:, bc], op=ALU.mult), r=[psk(bank), "ROPE"], w=[("T2", ii)])
                    sc.op("dve", lambda e, dst=dst, bc=bc, T1=T1, T2=T2: e.tensor_tensor(out=dst[:, bc], in0=T1, in1=T2, op=ALU.add), r=[("T1", ii), ("T2", ii)], w=[dkey])
        par = [0]
        for g in range(NB):
            gc = slice(g * 512, (g + 1) * 512)
            for m in range(2):
                base = 64 * m
                rows = slice(base, base + 64)
                drows = slice(64 - base, 128 - base)
                nj = 4 * g + 4
                ab = 2 + 2 * par[0]
                par[0] ^= 1
                head = 2 * u + m

                def score(j, g=g, rows=rows, head=head):
                    c0 = max(0, j - 4 * g) * 128
                    b = j % 3
                    sb = SBK[b]
                    qcols = slice(g * 512 + c0, (g + 1) * 512)
                    sc.op("pe", lambda e: e.matmul(PS[sb][:, c0:512], lhsT=KT[rows, j * 128:(j + 1) * 128], rhs=QT[rows, qcols], start=True, stop=(not is_fox)),
                          r=["KT", "QT"], w=[psk(sb)])
                    if is_fox:
                        sc.op("pe", lambda e: e.matmul(PS[sb][:, c0:512], lhsT=ESELB[0:72, head * 128:(head + 1) * 128], rhs=fox[0][0:72, qcols], start=False, stop=True),
                              r=["CB", "CAUG"], w=[psk(sb)])

                def rest(j, g=g, m=m, nj=nj, ab=ab, head=head):
                    c0 = max(0, j - 4 * g) * 128
                    b = j % 3
                    sb = SBK[b]
                    if is_fox:
                        sc.op("act", lambda e: e.activation(out=PT[b][:, c0:512], in_=PS[sb][:, c0:512], func=AF.Exp, bias=fox[1][:, j, head:head + 1], scale=1.0),
                              r=[psk(sb), "NEGC"], w=[("PT", b)])
                    else:
                        sc.op("act", lambda e: e.activation(out=PT[b][:, c0:512], in_=PS[sb][:, c0:512], func=AF.Exp, scale=0.125), r=[psk(sb)], w=[("PT", b)])
                    if j >= 4 * g:
                        sc.op("dve", lambda e: e.tensor_tensor(out=PT[b][:, c0:c0 + 128], in0=PT[b][:, c0:c0 + 128], in1=TRIB, op=ALU.mult), r=[("PT", b), "CB"], w=[("PT", b)])
                    if is_fox:
                        sc.op("pe", lambda e: e.matmul(PS[ab][:, c0:512], lhsT=VT2[:, j, m, :], rhs=PT[b][:, c0:512], start=(j == 0), stop=(j == nj - 1)),
                              r=["AVT", ("PT", b)], w=[psk(ab)])
                    else:
                        sc.op("pe", lambda e: e.matmul(PS[ab][:, c0:512], lhsT=VT[:, j, :], rhs=PT[b][:, c0:512], start=(j == 0), stop=(j == nj - 1)),
                              r=["AVT", ("PT", b)], w=[psk(ab)])
                        sc.op("pe", lambda e: e.matmul(PS[ab + 1][:, c0:512], lhsT=ONESB, rhs=PT[b][:, c0:512], start=(j == 0), stop=(j == nj - 1)),
                              r=["CB", ("PT", b)], w=[psk(ab + 1)])

                score(0)
                if nj > 1:
                    score(1)
                for j in range(nj):
                    if j + 2 < nj:
                        score(j + 2)
                    rest(j)
                if is_fox:
                    sc.op("dve", lambda e, rows=rows, drows=drows, ab=ab: e.reciprocal(out=RD[rows, :], in_=PS[ab][drows, :]), r=[psk(ab)], w=["RD"])
                    sc.op("dve", lambda e, rows=rows, gc=gc, ab=ab: e.tensor_tensor(out=YT[rows, 4 + u, gc], in0=PS[ab][rows, :], in1=RD[rows, :], op=ALU.mult),
                          r=[psk(ab), "RD"], w=[("YT", 4 + u)])
                else:
                    sc.op("dve", lambda e, ab=ab: e.reciprocal(out=RD, in_=PS[ab + 1][:, :]), r=[psk(ab + 1)], w=["RD"])
                    if m == 0:
                        sc.op("dve", lambda e, ab=ab: e.tensor_tensor(out=O0, in0=PS[ab][:, :], in1=RD, op=ALU.mult), r=[psk(ab), "RD"], w=["O0"])
                    else:
                        sc.op("dve", lambda e, ab=ab: e.tensor_tensor(out=OD, in0=PS[ab][:, :], in1=RD, op=ALU.mult), r=[psk(ab), "RD"], w=["OD"])
                        sc.op("dve", lambda e: e.scalar_tensor_tensor(out=OD, in0=OD, scalar=NLAM[:, l:l + 1], in1=O0, op0=ALU.mult, op1=ALU.add),
                              r=["OD", "O0", "SM"], w=["OD"])
                        rms_finish(OD, "OD", SQ, "ASQ", RS, "ARS", ab + 1)
                        sc.op("dve", lambda e: e.tensor_tensor(out=OD, in0=OD, in1=RS, op=ALU.mult), r=["OD", "ARS"], w=["OD"])
                        sc.op("dve", lambda e, gc=gc: e.tensor_scalar(out=YT[:, 8 + u, gc], in0=OD, scalar1=DNG[:, l:l + 1], scalar2=None, op0=ALU.mult),
                              r=["OD", "SM"], w=[("YT", 8 + u)])
        wk.release(mk)

    def fox_prep(l, wk, wk2):
        WFF = wtile(wk2, 72, "wff")
        sc.op("dve", lambda e: e.memset(WFF, 0.0), w=["wff"])
        for b0 in (0, 32, 64):
            sc.dma("pool", lambda e, b0=b0: e.dma_start(out=WFF[:, :, b0:b0 + 8], in_=wview(l, O_FF, O_FF + 8)), r=["wff"], w=["wff"])
        NCF = wk.f32(S, "ncf")
        CAUG = wk.bf16(S, "caug"); MIDB = wk.bf16(S, "midb"); LOB = wk.bf16(S, "lob")
        R1 = wk.f32(S, "r1")
        NEGC = wk.f32(NT * 8, "negc").rearrange("p (t h) -> p t h", t=NT)
        Z512 = wk2.f32(512, "z512"); E1 = wk2.f32(512, "e1")
        sc.op("dve", lambda e: e.memset(Z512, 0.0), w=["Z512"])
        sc.op("dve", lambda e: e.memset(NCF, 0.0), w=["NCF"])
        R = slice(0, 72)
        for blk in range(NB):
            bc = slice(blk * 512, (blk + 1) * 512)
            proj_fm(0, WFF, 72, blk, "wff")
            sc.op("act", lambda e: e.activation(out=E1[R, :], in_=PS[0][R, :], func=AF.Exp, bias=FB[R, l:l + 1], scale=-1.0), r=[psk(0), "SM"], w=["E1"])
            sc.op("act", lambda e: e.activation(out=E1[R, :], in_=E1[R, :], func=AF.Ln, bias=C_ONE[R, :], scale=1.0), r=["E1"] + CK, w=["E1"])
            init = 0.0 if blk == 0 else NCF[R, blk * 512 - 1:blk * 512]
            sc.op("dve", lambda e, bc=bc, init=init: e.tensor_tensor_scan(out=NCF[R, bc], data0=Z512[R, :], data1=E1[R, :], initial=init, op0=ALU.add, op1=ALU.add),
                  r=["E1", "Z512", "NCF"], w=["NCF"])
        sc.op("dve", lambda e: e.tensor_scalar(out=CAUG[R, :], in0=NCF[R, :], scalar1=-1.0, scalar2=None, op0=ALU.mult), r=["NCF"], w=["CAUG"])
        sc.op("dve", lambda e: e.scalar_tensor_tensor(out=R1[R, :], in0=NCF[R, :], scalar=-1.0, in1=CAUG[R, :], op0=ALU.mult, op1=ALU.subtract),
              r=["NCF", "CAUG"], w=["R1"])
        sc.op("dve", lambda e: e.tensor_copy(out=MIDB[R, :], in_=R1[R, :]), r=["R1"], w=["MIDB"])
        sc.op("dve", lambda e: e.tensor_tensor(out=R1[R, :], in0=R1[R, :], in1=MIDB[R, :], op=ALU.subtract), r=["R1", "MIDB"], w=["R1"])
        sc.op("dve", lambda e: e.tensor_copy(out=LOB[R, :], in_=R1[R, :]), r=["R1"], w=["LOB"])
        sc.op("dve", lambda e: e.tensor_copy(out=CAUG[32:40, :], in_=MIDB[32:40, :]), r=["MIDB", "CAUG"], w=["CAUG"])
        sc.op("dve", lambda e: e.tensor_copy(out=CAUG[64:72, :], in_=LOB[64:72, :]), r=["LOB", "CAUG"], w=["CAUG"])
        for T in range(NT):
            sc.op("pe", lambda e, T=T: e.transpose(out=PS[1][:, T * 8:(T + 1) * 8], in_=NCF[0:8, T * 128:(T + 1) * 128], identity=IDF[0:8, 0:8]),
                  r=["NCF", "CF"], w=[psk(1)])
        sc.op("dve", lambda e: e.tensor_copy(out=NEGC, in_=PS[1][:, 0:NT * 8].rearrange("p (t h) -> p t h", t=NT)), r=[psk(1)], w=["NEGC"])
        return CAUG, NEGC

    def rope_tables(q, wk, CT, ST_):
        POSI = wk.f32(S, "posi").bitcast(I32)
        U = wk.f32(S, "ropeu"); KF = wk.f32(S, "ropek"); KI = wk.f32(S, "ropeki").bitcast(I32)
        sc.dma("sp", lambda e: e.dma_start(out=POSI, in_=pos_in[q].partition_broadcast(128)), w=["POSI"])
        sc.op("dve", lambda e: e.tensor_copy(out=U, in_=POSI), r=["POSI"], w=["U"])
        sc.op("dve", lambda e: e.tensor_scalar(out=U, in0=U, scalar1=MISC[:, 0:1], scalar2=None, op0=ALU.mult), r=["U", "CF"], w=["U"])
        for (dst, off, key) in ((CT, 0.75, "c"), (ST_, 0.5, "s")):
            sc.op("dve", lambda e, dst=dst, off=off: e.tensor_scalar(out=dst, in0=U, scalar1=off, scalar2=None, op0=ALU.add), r=["U"], w=["ROPE"])
            sc.op("dve", lambda e, dst=dst: e.tensor_copy(out=KI, in_=dst), r=["ROPE"], w=["KI"])
            sc.op("dve", lambda e: e.tensor_copy(out=KF, in_=KI), r=["KI"], w=["KF"])
            sc.op("dve", lambda e, dst=dst: e.tensor_tensor(out=dst, in0=dst, in1=KF, op=ALU.subtract), r=["ROPE", "KF"], w=["ROPE"])
            sc.op("dve", lambda e, dst=dst: e.tensor_scalar(out=KF, in0=dst, scalar1=0.0, scalar2=None, op0=ALU.is_lt), r=["ROPE"], w=["KF"])
            sc.op("dve", lambda e, dst=dst: e.tensor_tensor(out=dst, in0=dst, in1=KF, op=ALU.add), r=["ROPE", "KF"], w=["ROPE"])
            sc.op("act", lambda e, dst=dst: e.activation(out=dst, in_=dst, func=AF.Sin, bias=C_SINB, scale=SINSC), r=["ROPE"] + CK, w=["ROPE"])
        sc.op("dve", lambda e: e.tensor_scalar(out=ST_, in0=ST_, scalar1=MISC[:, 1:2], scalar2=None, op0=ALU.mult), r=["ROPE", "CF"], w=["ROPE"])

    def dump_bf16(src3, nchunk, key_fn, wk):
        TMPD = wk.f32(S, "dumptmp")
        for c in range(nchunk):
            sc.op("dve", lambda e, c=c: e.tensor_copy(out=TMPD, in_=src3[:, c, :]), r=[key_fn(c)], w=["TMPD"])
            sc.dma("sp", lambda e, c=c: e.dma_start(out=dbg_out[c * 128:(c + 1) * 128, :], in_=TMPD), r=["TMPD"], w=["dbg"])

    stop = False
    for q in range(NSEQ):
        row0 = q * S
        sc.barrier()
        A_TL.release(0)
        load_ln(ln_in_g, ln_in_b)
        XB = [A_TL.bf16(D, "xb%d" % i) for i in range(2)]
        SMALL = A_TL.f32(16 * NT, "small")
        for t in range(NT):
            xa = ACC[:, t, :]
            sc.dma("sp", lambda e, t=t, xa=xa, row0=row0: e.dma_start(out=xa, in_=x_in[row0 + t * 128:row0 + (t + 1) * 128, :]), w=[("ACC", t)])

        def post0(t, xa, xk):
            sc.dma("sp", lambda e: e.dma_start(out=xres[t * 128:(t + 1) * 128, :], in_=xa), r=[xk], w=[("xres", t)])
        ln_and_xt([(t, ACC[:, t, :], ("ACC", t)) for t in range(NT)], SMALL, XB, XT, "XT", post=post0)

        for l in range(L):
            sc.barrier()
            A_TL.release(0); A_MT.release(0); A_ACC.release(yt_mark)
            CT = A_ACC.f32(S, "ropec"); ST_ = A_ACC.f32(S, "ropes")
            rope_tables(q, A_TL, CT, ST_)
            sc.barrier()
            A_TL.release(0)
            HW = [hgrn_walloc(A_MT if S >= 1024 else A_TL, par) for par in range(2)]
            hgrn_load(l, 0, HW[0])
            for h in range(4):
                if h + 1 < 4:
                    hgrn_load(l, h + 1, HW[(h + 1) % 2])
                hgrn_unit(l, h, A_TL, HW[h % 2])
            sc.barrier()
            A_TL.release(0)
            A_MT.release(0)
            foxp = fox_prep(l, A_MT, A_TL)
            sc.barrier()
            A_TL.release(0)
            AW = [attn_walloc(A_TL, par, 3) for par in range(2)]
            attn_load(l, "fox", 0, AW[0])
            for p in range(4):
                if p + 1 < 4:
                    attn_load(l, "fox", p + 1, AW[(p + 1) % 2])
                attn_unit(l, "fox", p, A_TL, AW[p % 2], fox=foxp)
            sc.barrier()
            A_TL.release(0); A_MT.release(0)
            AW = [attn_walloc(A_TL, par, 5) for par in range(2)]
            attn_load(l, "diff", 0, AW[0])
            for h in range(4):
                if h + 1 < 4:
                    attn_load(l, "diff", h + 1, AW[(h + 1) % 2])
                attn_unit(l, "diff", h, A_TL, AW[h % 2], rope=(CT, ST_))
            if dbg is not None and dbg[0] == "yt":
                sc.barrier()
                A_TL.release(0)
                dump_bf16(YT, 12, lambda c: ("YT", c), A_TL)
                stop = True
                break
            sc.barrier()
            A_TL.release(0); A_MT.release(0)
            WGt = [A_TL.bf16(KC * 3 * 128, "wgt%d" % i).rearrange("p (k r n) -> p k r n", k=KC, r=3) for i in range(2)]
            WBt = [A_TL.bf16(3 * 4 * 128, "wbt%d" % i).rearrange("p (r w n) -> p r w n", r=3, w=4) for i in range(2)]
            SIG = [A_TL.f32(512, "sig%d" % i) for i in range(3)]
            MM = A_TL.f32(512, "mm"); MT2 = A_TL.f32(512, "mt2")
            for dc in range(KC):
                b = dc % 2
                for r in range(3):
                    o = O_GT + r * 1024 + dc * 128
                    load_w(WGt[b][:, :, r, :], wview(l, o, o + 128), ("wgt", b))
                    load_w(WBt[b][:, r, :, :], w_branch[l, r].rearrange("(w p) d -> p w d", p=128)[:, :, dc * 128:(dc + 1) * 128], ("wbt", b))
                for blk in range(NB):
                    bc = slice(blk * 512, (blk + 1) * 512)
                    for r in range(3):
                        proj_fm(r, WGt[b][:, :, r, :], 128, blk, ("wgt", b))
                        sc.op("act", lambda e, r=r: e.activation(out=SIG[r], in_=PS[r][:, :], func=AF.Sigmoid), r=[psk(r)], w=[("SIG", r)])
                        for wc in range(4):
                            sc.op("pe", lambda e, r=r, wc=wc, b=b, bc=bc: e.matmul(PS[3 + r][:, :], lhsT=WBt[b][:, r, wc, :], rhs=YT[:, r * 4 + wc, bc],
                                                                                start=(wc == 0), stop=(wc == 3)),
                                  r=[("wbt", b), ("YT", r * 4 + wc)], w=[psk(3 + r)])
                    sc.op("dve", lambda e: e.tensor_tensor(out=MM, in0=SIG[0], in1=PS[3][:, :], op=ALU.mult), r=[("SIG", 0), psk(3)], w=["MM"])
                    sc.op("dve", lambda e: e.tensor_tensor(out=MT2, in0=SIG[1], in1=PS[4][:, :], op=ALU.mult), r=[("SIG", 1), psk(4)], w=["MT2"])
                    sc.op("dve", lambda e: e.tensor_tensor(out=MM, in0=MM, in1=MT2, op=ALU.add), r=["MM", "MT2"], w=["MM"])
                    sc.op("dve", lambda e: e.tensor_tensor(out=MT2, in0=SIG[2], in1=PS[5][:, :], op=ALU.mult), r=[("SIG", 2), psk(5)], w=["MT2"])
                    sc.op("dve", lambda e, dc=dc, bc=bc: e.tensor_tensor(out=MT[:, dc, bc], in0=MM, in1=MT2, op=ALU.add), r=["MM", "MT2"], w=["MTw"])

            sc.barrier()
            A_TL.release(0); A_ACC.release(0); A_XT.release(0)
            load_ln(ln1_g[l], ln1_b[l])
            big_xt = (KC * S // 2) >= 6144 + 2048
            EW0 = None
            if big_xt:
                EW0 = (wtile(A_XT, 1024, "ewgu0"), A_XT.bf16(4 * 1024, "ewd0").rearrange("p (k n) -> p k n", k=4))

            def load_expert(ex, gu, dn, key):
                load_w(gu[:, :, 0:512], ew_g[l, ex].rearrange("(k p) n -> p k n", p=128), key)
                load_w(gu[:, :, 512:1024], ew_u[l, ex].rearrange("(k p) n -> p k n", p=128), key)
                load_w(dn, ew_d[l, ex].rearrange("(k p) n -> p k n", p=128), key)
            if big_xt:
                load_expert(0, EW0[0], EW0[1], ("ew", 0))
            WO = wtile(A_TL, 1024, "wo")
            load_w(WO, w_out[l].rearrange("(k p) n -> p k n", p=128), "wo")
            WR = wtile(A_TL, 36, "wr")
            load_w(WR[:, :, 0:4], rg_w[l].rearrange("(k p) n -> p k n", p=128), "wr")
            load_w(WR[:, :, 4:36], re_w[l].rearrange("(k p) n -> p k n", p=128), "wr")
            RB = A_TL.f32(36, "rb")
            sc.dma("sp", lambda e: e.dma_start(out=RB[:, 0:4], in_=rg_b[l].partition_broadcast(128)), w=["RB"])
            sc.dma("sp", lambda e: e.dma_start(out=RB[:, 4:36], in_=re_b[l].partition_broadcast(128)), w=["RB"])
            XR = [A_TL.f32(D, "xr%d" % i) for i in range(2)]
            XB = [A_TL.bf16(D, "xb%d" % i) for i in range(2)]
            SMALL = A_TL.f32(16 * NT, "small")
            RWA = A_TL.f32(96 * NT, "rwa")
            for t in range(NT):
                tc_ = slice(t * 128, (t + 1) * 128)
                xa = ACC[:, t, :]
                xr = XR[t % 2]
                sc.dma("sp", lambda e, t=t, xr=xr: e.dma_start(out=xr, in_=xres[t * 128:(t + 1) * 128, :]), r=[("xres", t)], w=[("xr", t % 2)])
                for hf in range(2):
                    bank = (t % 2) * 2 + hf
                    for kc in range(KC):
                        sc.op("pe", lambda e, hf=hf, kc=kc, tc_=tc_, bank=bank: e.matmul(PS[bank][:, :], lhsT=MT[:, kc, tc_], rhs=WO[:, kc, hf * 512:(hf + 1) * 512],
                                                                                     start=(kc == 0), stop=(kc == KC - 1)),
                              r=["wo", ("MT", t), "MTw"], w=[psk(bank)])
                    sc.op("dve", lambda e, hf=hf, xa=xa, xr=xr, bank=bank: e.scalar_tensor_tensor(out=xa[:, hf * 512:(hf + 1) * 512], in0=xr[:, hf * 512:(hf + 1) * 512], scalar=float(ALPHA),
                                                                                              in1=PS[bank][:, :], op0=ALU.mult, op1=ALU.add),
                          r=[("xr", t % 2), psk(bank)], w=[("ACC", t)])

            def post1(t, xa, xk):
                if dbg is not None and dbg[0] == "x1" and l == 0:
                    sc.dma("sp", lambda e: e.dma_start(out=dbg_out[t * 128:(t + 1) * 128, :], in_=xa), r=[xk], w=["dbg"])
            ln_and_xt([(t, ACC[:, t, :], ("ACC", t)) for t in range(NT)], SMALL, XB, MT, "MT", post=post1)
            for t in range(NT):
                sc.op("dve", lambda e, t=t: e.tensor_scalar(out=ACC[:, t, :], in0=ACC[:, t, :], scalar1=float(ALPHA), scalar2=None, op0=ALU.mult), r=[("ACC", t)], w=[("ACC", t)])
            for t in range(NT):
                tc_ = slice(t * 128, (t + 1) * 128)
                bank = 4 + t // 8
                c0 = (t % 8) * 36
                for kc in range(KC):
                    sc.op("pe", lambda e, kc=kc, tc_=tc_, bank=bank, c0=c0: e.matmul(PS[bank][:, c0:c0 + 36], lhsT=MT[:, kc, tc_], rhs=WR[:, kc, :], start=(kc == 0), stop=(kc == KC - 1)),
                          r=["wr", ("MT", t)], w=[psk(bank)])

            def rw(t, a, b):
                return RWA[:, t * 96 + a:t * 96 + b]

            def stage(fn, eng="dve", extra_r=()):
                for t in range(NT):
                    sc.op(eng, (lambda e, t=t: fn(e, t)), r=[("RW", t)] + list(extra_r(t) if callable(extra_r) else extra_r), w=[("RW", t)])
            LGT = lambda t: rw(t, 0, 36)
            GM = lambda t: rw(t, 36, 37); NGM = lambda t: rw(t, 37, 38); EG = lambda t: rw(t, 38, 42); SE = lambda t: rw(t, 42, 43)
            GP = lambda t: rw(t, 43, 44); OH = lambda t: rw(t, 44, 48); ESL = lambda t: rw(t, 48, 56); MX8 = lambda t: rw(t, 56, 64)
            K1 = lambda t: rw(t, 64, 72); K2 = lambda t: rw(t, 72, 80); DM = lambda t: rw(t, 80, 81); EX = lambda t: rw(t, 81, 82)
            W1 = lambda t: rw(t, 82, 83); W2 = lambda t: rw(t, 83, 84); GL = lambda t: rw(t, 84, 92)
            stage(lambda e, t: e.tensor_tensor(out=LGT(t), in0=PS[4 + t // 8][:, (t % 8) * 36:(t % 8) * 36 + 36], in1=RB, op=ALU.add),
                  extra_r=lambda t: [psk(4 + t // 8), "RB"])
            stage(lambda e, t: e.tensor_reduce(out=GM(t), in_=LGT(t)[:, 0:4], axis=AX.X, op=ALU.max))
            stage(lambda e, t: e.tensor_scalar(out=NGM(t), in0=GM(t), scalar1=-1.0, scalar2=None, op0=ALU.mult))
            stage(lambda e, t: e.activation(out=EG(t), in_=LGT(t)[:, 0:4], func=AF.Exp, bias=NGM(t), scale=1.0), eng="act")
            stage(lambda e, t: e.tensor_reduce(out=SE(t), in_=EG(t), axis=AX.X, op=ALU.add))
            stage(lambda e, t: e.reciprocal(out=GP(t), in_=SE(t)))
            stage(lambda e, t: e.tensor_scalar(out=OH(t), in0=LGT(t)[:, 0:4], scalar1=GM(t), scalar2=None, op0=ALU.is_equal))
            stage(lambda e, t: e.tensor_scalar(out=ESL(t), in0=LGT(t)[:, 4:12], scalar1=OH(t)[:, 0:1], scalar2=None, op0=ALU.mult))
            for g in range(1, 4):
                stage(lambda e, t, g=g: e.scalar_tensor_tensor(out=ESL(t), in0=LGT(t)[:, 4 + 8 * g:12 + 8 * g], scalar=OH(t)[:, g:g + 1], in1=ESL(t), op0=ALU.mult, op1=ALU.add))
            stage(lambda e, t: e.max(out=MX8(t), in_=ESL(t)))
            stage(lambda e, t: e.tensor_scalar(out=K1(t), in0=ESL(t), scalar1=MX8(t)[:, 0:1], scalar2=None, op0=ALU.is_equal))
            stage(lambda e, t: e.tensor_scalar(out=K2(t), in0=ESL(t), scalar1=MX8(t)[:, 1:2], scalar2=None, op0=ALU.is_equal))
            stage(lambda e, t: e.tensor_tensor(out=DM(t), in0=MX8(t)[:, 1:2], in1=MX8(t)[:, 0:1], op=ALU.subtract))
            stage(lambda e, t: e.activation(out=EX(t), in_=DM(t), func=AF.Exp), eng="act")
            stage(lambda e, t: e.tensor_scalar(out=W1(t), in0=EX(t), scalar1=1.0, scalar2=None, op0=ALU.add))
            stage(lambda e, t: e.reciprocal(out=W1(t), in_=W1(t)))
            stage(lambda e, t: e.tensor_tensor(out=W2(t), in0=EX(t), in1=W1(t), op=ALU.mult))
            stage(lambda e, t: e.tensor_tensor(out=W1(t), in0=W1(t), in1=GP(t), op=ALU.mult))
            stage(lambda e, t: e.tensor_tensor(out=W2(t), in0=W2(t), in1=GP(t), op=ALU.mult))
            stage(lambda e, t: e.tensor_scalar(out=GL(t), in0=K1(t), scalar1=W1(t), scalar2=None, op0=ALU.mult))
            stage(lambda e, t: e.scalar_tensor_tensor(out=GL(t), in0=K2(t), scalar=W2(t), in1=GL(t), op0=ALU.mult, op1=ALU.add))
            for g in range(4):
                for t in range(NT):
                    sc.op("dve", lambda e, g=g, t=t: e.tensor_scalar(out=GATE[:, t, g * 8:(g + 1) * 8], in0=GL(t), scalar1=OH(t)[:, g:g + 1], scalar2=None, op0=ALU.mult),
                          r=[("RW", t)], w=["GATE"])
            if dbg is not None and dbg[0] == "x1" and l == 0:
                stop = True
                break

            sc.barrier()
            A_TL.release(0)
            if not big_xt:
                A_XT.release(0)
                EW0 = (wtile(A_TL, 1024, "ewgu0"), A_TL.bf16(4 * 1024, "ewd0").rearrange("p (k n) -> p k n", k=4))
                load_expert(0, EW0[0], EW0[1], ("ew", 0))
            EWGU = [EW0[0], wtile(A_TL, 1024, "ewgu1")]
            EWD = [EW0[1], A_TL.bf16(4 * 1024, "ewd1").rearrange("p (k n) -> p k n", k=4)]
            HA = A_XT
            HTs = [A_TL.bf16(4 * 512, "ht%d" % i).rearrange("p (f t) -> p f t", f=4) for i in range(2)]
            SGT = [HA.f32(512, "sgt%d" % i) for i in range(2)]
            for ex in range(NEXP):
                b = ex % 2
                if ex > 0:
                    load_expert(ex, EWGU[b], EWD[b], ("ew", b))
                for blk in range(NB):
                    HT = HTs[blk % 2]
                    hk = ("HT", blk % 2)
                    for fc in range(4):
                        bg = (fc % 2) * 2
                        proj_fm(bg, EWGU[b][:, :, fc * 128:(fc + 1) * 128], 128, blk, ("ew", b), xt=MT, xkey="MT")
                        proj_fm(bg + 1, EWGU[b][:, :, 512 + fc * 128:512 + (fc + 1) * 128], 128, blk, ("ew", b), xt=MT, xkey="MT")
                        sc.op("act", lambda e, fc=fc, bg=bg: e.activation(out=SGT[fc % 2], in_=PS[bg][:, :], func=AF.Silu), r=[psk(bg)], w=[("SGT", fc % 2)])
                        sc.op("dve", lambda e, fc=fc, bg=bg, HT=HT: e.tensor_tensor(out=HT[:, fc, :], in0=SGT[fc % 2], in1=PS[bg + 1][:, :], op=ALU.mult),
                              r=[("SGT", fc % 2), psk(bg + 1)], w=[hk])
                    for tt in range(4):
                        T = blk * 4 + tt
                        for hf in range(2):
                            bank = 4 + (tt * 2 + hf) % 4
                            for fc in range(4):
                                sc.op("pe", lambda e, bank=bank, fc=fc, tt=tt, hf=hf, b=b, HT=HT: e.matmul(PS[bank][:, :], lhsT=HT[:, fc, tt * 128:(tt + 1) * 128],
                                                                                                          rhs=EWD[b][:, fc, hf * 512:(hf + 1) * 512], start=(fc == 0), stop=(fc == 3)),
                                      r=[hk, ("ew", b)], w=[psk(bank)])
                            sc.op("dve", lambda e, bank=bank, T=T, hf=hf, ex=ex: e.scalar_tensor_tensor(out=ACC[:, T, hf * 512:(hf + 1) * 512], in0=PS[bank][:, :],
                                                                                                     scalar=GATE[:, T, ex:ex + 1], in1=ACC[:, T, hf * 512:(hf + 1) * 512],
                                                                                                     op0=ALU.mult, op1=ALU.add),
                                  r=[psk(bank), "GATE", ("ACC", T)], w=[("ACC", T)])

            sc.barrier()
            A_TL.release(0); A_XT.release(0)
            load_ln(ln2_g[l], ln2_b[l])
            XB = [A_TL.bf16(D, "xb%d" % i) for i in range(2)]
            SMALL = A_TL.f32(16 * NT, "small")
            last = (l == L - 1)
            is_dbg2 = dbg is not None and dbg[0] == "x2" and l == 0

            def post2(t, xa, xk, last=last, is_dbg2=is_dbg2, row0=row0):
                if is_dbg2:
                    sc.dma("sp", lambda e: e.dma_start(out=dbg_out[t * 128:(t + 1) * 128, :], in_=xa), r=[xk], w=["dbg"])
                elif last:
                    sc.dma("sp", lambda e: e.dma_start(out=y_out[row0 + t * 128:row0 + (t + 1) * 128, :], in_=xa), r=[xk], w=[("y", t)])
                else:
                    sc.dma("sp", lambda e: e.dma_start(out=xres[t * 128:(t + 1) * 128, :], in_=xa), r=[xk], w=[("xres", t)])
            ln_and_xt([(t, ACC[:, t, :], ("ACC", t)) for t in range(NT)], SMALL, XB, (None if (last or is_dbg2) else XT), "XT", post=post2)
            if dbg is not None and dbg[0] == "x2" and l == 0:
                stop = True
                break
        if stop:
            break

    sc.emit(nc, es)
    es.close()
    return nc


def make_w_sw(w_in):
    L = w_in.shape[0]
    perm64 = np.concatenate([np.arange(8, 16), np.arange(0, 8), np.arange(16, 64)])
    out = np.empty((L, D, 1024), np.float32)
    for i, o in enumerate((O_DQ, O_DK)):
        blk = w_in[:, :, o:o + 512].reshape(L, D, 8, 64)[:, :, :, perm64]
        out[:, :, i * 512:(i + 1) * 512] = blk.reshape(L, D, 512)
    return out


_W_KEYS = ["ln_in_g", "ln_in_b", "w_in", "hgrn_lb_logits", "hgrn_norm_g", "fox_f_bias", "diff_lambda", "diff_norm_g",
           "w_branch", "w_out", "ln1_g", "ln1_b", "router_g_w", "router_g_b", "router_e_w",
           "expert_w_gate", "expert_w_up", "expert_w_down", "ln2_g", "ln2_b"]


def kernel(**inputs):
    x = np.ascontiguousarray(np.asarray(inputs["x"], dtype=np.float32))
    pos = np.ascontiguousarray(np.asarray(inputs["positions"]).astype(np.int32))
    B, S, Dm = x.shape
    ncores = 8
    per = B // ncores
    shared = {k: np.ascontiguousarray(np.asarray(inputs[k], dtype=np.float32)) for k in _W_KEYS}
    shared["router_e_b"] = np.ascontiguousarray(np.asarray(inputs["router_e_b"], dtype=np.float32).reshape(DEPTH, 32))
    shared["w_sw"] = make_w_sw(shared["w_in"])
    shared["consts"] = CONST_ARR
    nc = build_program(S, per)
    in_maps = []
    for c in range(ncores):
        m = dict(shared)
        m["x"] = x[c * per:(c + 1) * per].reshape(per * S, Dm)
        m["pos"] = pos[c * per:(c + 1) * per]
        in_maps.append(m)
    res = run_bass_kernel_spmd(nc, in_maps, core_ids=list(range(ncores)))
    outs = [np.asarray(r["y"], dtype=np.float32).reshape(per, S, Dm) for r in res.results]
    return np.concatenate(outs, axis=0)
```

```python
import math
from contextlib import ExitStack
import numpy as np
import concourse.bass as bass
import concourse.mybir as mybir
from concourse.bass_utils import run_bass_kernel_spmd

F32 = mybir.dt.float32
BF16 = mybir.dt.bfloat16
I32 = mybir.dt.int32
AF = mybir.ActivationFunctionType
ALU = mybir.AluOpType
AX = mybir.AxisListType

D = 1024
KC = 8
NIN = 8200
DEPTH = 2
NEXP = 32
DEXP = 512
ALPHA = (2 * DEPTH) ** 0.25
LN_EPS = 1e-5
RMS_EPS = 1e-6
O_HQ, O_HF, O_HI, O_HG = 0, 512, 1024, 1536
O_FQ, O_FK, O_FV, O_FF = 2048, 2560, 3072, 3584
O_DQ, O_DK, O_DV, O_GT = 3592, 4104, 4616, 5128
NDMASEM = 8


class Sched:
    ENGS = ("pe", "act", "dve", "pool", "sp")

    def __init__(self):
        self.q = {e: [] for e in self.ENGS}
        self.cnt = {e: 0 for e in self.ENGS}
        self.dcnt = {"sp": 0, "pool": 0}
        self.known = {e: {} for e in self.ENGS}
        self.w = {}
        self.r = {}
        self.all_tokens = {}

    def _deps(self, eng, reads, writes):
        deps = {}

        def add(tok):
            if tok is None:
                return
            k, v = tok
            if eng == "pe" and k == "pe":
                return
            if deps.get(k, 0) < v:
                deps[k] = v

        for r in reads:
            add(self.w.get(r))
        for r in writes:
            add(self.w.get(r))
            for tok in self.r.get(r, {}).items():
                add(tok)
        return deps

    def _commit(self, tok, reads, writes):
        for r in reads:
            d = self.r.setdefault(r, {})
            if d.get(tok[0], 0) < tok[1]:
                d[tok[0]] = tok[1]
        for r in writes:
            self.w[r] = tok
            self.r[r] = {}
        self.all_tokens[tok[0]] = max(self.all_tokens.get(tok[0], 0), tok[1])

    def _waits(self, eng, deps):
        waits = []
        kn = self.known[eng]
        for k, v in deps.items():
            if kn.get(k, 0) < v:
                waits.append((k, v))
                kn[k] = v
        return waits

    def op(self, eng, fn, r=(), w=()):
        deps = self._deps(eng, r, w)
        waits = self._waits(eng, deps)
        self.cnt[eng] += 1
        tok = (eng, self.cnt[eng])
        self.q[eng].append((waits, fn, (eng, 1)))
        self._commit(tok, r, w)

    def dma(self, qeng, fn, r=(), w=()):
        deps = self._deps(qeng, r, w)
        n = self.dcnt[qeng]
        self.dcnt[qeng] += 1
        key = ("dma", qeng, n % NDMASEM)
        val = 16 * (n // NDMASEM + 1)
        if val > 16:
            deps[key] = max(deps.get(key, 0), val - 16)
        waits = self._waits(qeng, deps)
        self.q[qeng].append((waits, fn, (key, 16)))
        self._commit((key, val), r, w)

    def barrier(self):
        for e in self.ENGS:
            waits = self._waits(e, dict(self.all_tokens))
            if waits:
                self.q[e].append((waits, None, None))

    def emit(self, nc, es):
        sems = {}
        for e in self.ENGS:
            sems[e] = es.enter_context(nc.semaphore("s_" + e))
        for qe in ("sp", "pool"):
            for i in range(NDMASEM):
                sems[("dma", qe, i)] = es.enter_context(nc.semaphore("d_%s%d" % (qe, i)))
        block = es.enter_context(nc.Block())

        def run(ename, eobj):
            for waits, fn, inc in self.q[ename]:
                for k, v in waits:
                    eobj.wait_ge(sems[k], v)
                if fn is not None:
                    ins = fn(eobj)
                    ins.then_inc(sems[inc[0]], inc[1])

        @block.tensor
        def _(e):
            run("pe", e)

        @block.scalar
        def _(e):
            run("act", e)

        @block.vector
        def _(e):
            run("dve", e)

        @block.gpsimd
        def _(e):
            run("pool", e)
            for k, v in self.all_tokens.items():
                e.wait_ge(sems[k], v)

        @block.sync
        def _(e):
            run("sp", e)
            for k, v in self.all_tokens.items():
                e.wait_ge(sems[k], v)


class Arena:
    def __init__(self, ap, nwords):
        self.ap = ap
        self.n = nwords
        self.top = 0
        self.uid = 0

    def mark(self):
        return self.top

    def release(self, m):
        self.top = m

    def f32(self, n, name):
        a = self.top
        self.top += n
        assert self.top <= self.n, "SBUF arena overflow at %s: %d > %d" % (name, self.top, self.n)
        return self.ap[:, a:a + n]

    def bf16(self, n, name):
        nw = (n + 1) // 2
        return self.f32(nw, name).bitcast(BF16)[:, 0:n]


def host_consts():
    c = {}
    c["ident"] = np.eye(128, dtype=np.float32)
    s = np.arange(128)[:, None]
    t = np.arange(128)[None, :]
    c["tri"] = (s <= t).astype(np.float32)
    c["trih"] = ((s <= t) & ((s // 64) == (t // 64))).astype(np.float32)
    c["ones"] = np.ones((128, 128), np.float32)
    rm = np.ones((128, 512), np.float32)
    rm[:, 0::64] = 0.0
    c["rmask"] = rm
    es = np.zeros((128, 8, 128), np.float32)
    for h in range(8):
        for b in (0, 32, 64):
            es[b + h, h, :] = 1.0
    c["esel"] = es.reshape(128, 1024)
    p = np.arange(128)
    d = p % 64
    invf = np.where(d < 16, 500000.0 ** (-(2.0 * (d % 8)) / 16.0), 0.0)
    sgn = np.where(d < 8, -1.0, np.where(d < 16, 1.0, 0.0))
    misc = np.zeros((128, 8), np.float32)
    misc[:, 0] = invf / (2.0 * math.pi)
    misc[:, 1] = sgn
    c["misc"] = misc
    order = ["ident", "tri", "trih", "ones", "rmask", "esel", "misc"]
    offs = {}
    o = 0
    for k in order:
        offs[k] = (o, c[k].shape[1])
        o += c[k].shape[1]
    arr = np.concatenate([c[k] for k in order], axis=1).astype(np.float32)
    return arr, offs


CONST_ARR, CONST_OFF = host_consts()
NCONST = CONST_ARR.shape[1]


def build_program(S, NSEQ, L=DEPTH, dbg=None):
    global LAST_SCHED
    NT = S // 128
    NB = S // 512
    nc = bass.Bass("TRN2", target_bir_lowering=False)
    dram = {}

    def din(name, shape, dt=F32):
        dram[name] = nc.dram_tensor(name, list(shape), dt, kind="ExternalInput").ap()
        return dram[name]

    x_in = din("x", [NSEQ * S, D])
    pos_in = din("pos", [NSEQ, S], I32)
    consts_in = din("consts", [128, NCONST])
    ln_in_g = din("ln_in_g", [D]); ln_in_b = din("ln_in_b", [D])
    w_in = din("w_in", [L, D, NIN])
    w_sw = din("w_sw", [L, D, 1024])
    lb_logits = din("hgrn_lb_logits", [L, 512])
    hg_norm = din("hgrn_norm_g", [L, 128])
    fox_bias = din("fox_f_bias", [L, 8])
    dlam = din("diff_lambda", [L, 4, 64])
    dnorm = din("diff_norm_g", [L, 128])
    w_branch = din("w_branch", [L, 3, 512, D])
    w_out = din("w_out", [L, D, D])
    ln1_g = din("ln1_g", [L, D]); ln1_b = din("ln1_b", [L, D])
    rg_w = din("router_g_w", [L, D, 4]); rg_b = din("router_g_b", [L, 4])
    re_w = din("router_e_w", [L, D, 32]); re_b = din("router_e_b", [L, 32])
    ew_g = din("expert_w_gate", [L, NEXP, D, DEXP])
    ew_u = din("expert_w_up", [L, NEXP, D, DEXP])
    ew_d = din("expert_w_down", [L, NEXP, DEXP, D])
    ln2_g = din("ln2_g", [L, D]); ln2_b = din("ln2_b", [L, D])
    y_out = nc.dram_tensor("y", [NSEQ * S, D], F32, kind="ExternalOutput").ap()
    xres = nc.dram_tensor("xres_scratch", [S, D], F32).ap()
    dbg_out = None
    if dbg is not None:
        dbg_out = nc.dram_tensor("dbg", list(dbg[1]), F32, kind="ExternalOutput").ap()

    sc = Sched()
    LAST_SCHED = sc
    es = ExitStack()
    NW = 52992
    arena_t = es.enter_context(nc.sbuf_tensor("arena", [128, NW], F32))
    ar = Arena(arena_t, NW)
    PS = [es.enter_context(nc.psum_tensor("ps%d" % i, [128, 512], F32)) for i in range(8)]

    def psk(i):
        return ("ps", i)

    CF = ar.f32(NCONST, "consts")
    sc.dma("sp", lambda e: e.dma_start(out=CF, in_=consts_in[:, :]), w=["CF"])

    def cf(name):
        o, n = CONST_OFF[name]
        return CF[:, o:o + n]

    IDF = cf("ident")
    ONESF = cf("ones")
    RMASK = cf("rmask")
    MISC = cf("misc")
    CB = ar.bf16(128 * 4 + 1024, "constbf")
    IDB = CB[:, 0:128]; TRIB = CB[:, 128:256]; TRIHB = CB[:, 256:384]; ONESB = CB[:, 384:512]
    ESELB = CB[:, 512:1536]
    sc.op("dve", lambda e: e.tensor_copy(out=CB[:, 0:512], in_=CF[:, 0:512]), r=["CF"], w=["CB"])
    o_es = CONST_OFF["esel"][0]
    sc.op("dve", lambda e: e.tensor_copy(out=ESELB, in_=CF[:, o_es:o_es + 1024]), r=["CF"], w=["CB"])

    SM = ar.f32(64, "small")
    smc = [0]

    def small(n):
        a = smc[0]
        smc[0] += n
        assert smc[0] <= 64
        return SM[:, a:a + n]

    LBL = small(L * 4)
    LB = small(L * 4)
    OML = small(L * 4)
    NOML = small(L * 4)
    HGN = small(L)
    DNG = small(L)
    NLAM = small(L)
    FB = small(L)
    TMP4 = small(8)
    sc.dma("sp", lambda e: e.dma_start(out=LBL.rearrange("p (l h) -> p l h", l=L),
                                       in_=lb_logits.rearrange("l (h c) -> c l h", c=128), allow_slow_non_contiguous=True), w=["SM"])
    sc.dma("sp", lambda e: e.dma_start(out=HGN, in_=hg_norm.rearrange("l v -> v l"), allow_slow_non_contiguous=True), w=["SM"])
    sc.dma("sp", lambda e: e.dma_start(out=DNG, in_=dnorm.rearrange("l v -> v l"), allow_slow_non_contiguous=True), w=["SM"])
    sc.op("dve", lambda e: e.memset(FB, 0.0), w=["SM"])
    for b0 in (0, 32, 64):
        sc.dma("sp", lambda e, b0=b0: e.dma_start(out=FB[b0:b0 + 8, :], in_=fox_bias.rearrange("l h -> h l"), allow_slow_non_contiguous=True),
               r=["SM"], w=["SM"])
    sc.op("dve", lambda e: e.tensor_scalar(out=FB, in0=FB, scalar1=-1.0, scalar2=None, op0=ALU.mult), r=["SM"], w=["SM"])
    EXL = small(L * 4)
    sc.op("act", lambda e: e.activation(out=EXL, in_=LBL, func=AF.Exp), r=["SM"], w=["SM"])
    SUM4 = TMP4[:, 0:4]
    sc.op("dve", lambda e: e.tensor_tensor(out=SUM4, in0=EXL[:, 0:4], in1=EXL[:, 4:8], op=ALU.add), r=["SM"], w=["SM"])
    sc.op("dve", lambda e: e.reciprocal(out=SUM4, in_=SUM4), r=["SM"], w=["SM"])
    SMX = small(L * 4)
    for l in range(L):
        sc.op("dve", lambda e, l=l: e.tensor_tensor(out=SMX[:, l * 4:l * 4 + 4], in0=EXL[:, l * 4:l * 4 + 4], in1=SUM4, op=ALU.mult),
              r=["SM"], w=["SM"])
    sc.op("dve", lambda e: e.tensor_tensor(out=LB[:, 0:4], in0=SMX[:, 0:4], in1=SMX[:, 0:4], op=ALU.subtract), r=["SM"], w=["SM"])
    sc.op("dve", lambda e: e.tensor_tensor(out=LB[:, 4:8], in0=SMX[:, 0:4], in1=SMX[:, 4:8], op=ALU.add), r=["SM"], w=["SM"])
    sc.op("dve", lambda e: e.tensor_tensor(out=LB[:, 4:8], in0=LB[:, 4:8], in1=SMX[:, 0:4], op=ALU.subtract), r=["SM"], w=["SM"])
    sc.op("dve", lambda e: e.tensor_scalar(out=LB, in0=LB, scalar1=0.0, scalar2=None, op0=ALU.max), r=["SM"], w=["SM"])
    sc.op("dve", lambda e: e.tensor_scalar(out=OML, in0=LB, scalar1=-1.0, scalar2=1.0, op0=ALU.mult, op1=ALU.add), r=["SM"], w=["SM"])
    sc.op("dve", lambda e: e.tensor_scalar(out=NOML, in0=OML, scalar1=-1.0, scalar2=None, op0=ALU.mult), r=["SM"], w=["SM"])
    LV = ar.f32(L * 256, "lamvec")
    sc.dma("sp", lambda e: e.dma_start(out=LV, in_=dlam.rearrange("l a d -> (l a d)").partition_broadcast(128)), w=["LV"])
    for l in range(L):
        lam_init = 0.8 - 0.6 * float(np.exp(-0.3 * l))
        base = l * 256
        P1 = LV[:, base:base + 64]
        P2 = LV[:, base + 128:base + 192]
        sc.op("dve", lambda e, P1=P1, base=base: e.tensor_tensor(out=P1, in0=P1, in1=LV[:, base + 64:base + 128], op=ALU.mult), r=["LV"], w=["LV"])
        sc.op("dve", lambda e, P2=P2, base=base: e.tensor_tensor(out=P2, in0=P2, in1=LV[:, base + 192:base + 256], op=ALU.mult), r=["LV"], w=["LV"])
        sc.op("dve", lambda e, P1=P1: e.tensor_reduce(out=TMP4[:, 4:5], in_=P1, axis=AX.X, op=ALU.add), r=["LV", "SM"], w=["SM"])
        sc.op("dve", lambda e, P2=P2: e.tensor_reduce(out=TMP4[:, 5:6], in_=P2, axis=AX.X, op=ALU.add), r=["LV", "SM"], w=["SM"])
        sc.op("act", lambda e: e.activation(out=TMP4[:, 4:6], in_=TMP4[:, 4:6], func=AF.Exp), r=["SM"], w=["SM"])
        sc.op("dve", lambda e: e.tensor_tensor(out=TMP4[:, 6:7], in0=TMP4[:, 5:6], in1=TMP4[:, 4:5], op=ALU.subtract), r=["SM"], w=["SM"])
        sc.op("dve", lambda e, l=l, lam_init=lam_init: e.tensor_scalar(out=NLAM[:, l:l + 1], in0=TMP4[:, 6:7], scalar1=-lam_init, scalar2=None, op0=ALU.add), r=["SM"], w=["SM"])
        sc.op("dve", lambda e, l=l, lam_init=lam_init: e.tensor_scalar(out=DNG[:, l:l + 1], in0=DNG[:, l:l + 1], scalar1=1.0 - lam_init, scalar2=None, op0=ALU.mult), r=["SM"], w=["SM"])

    XT = ar.bf16(KC * S, "XT").rearrange("p (k t) -> p k t", k=KC)
    xt_lo = ar.top - KC * S // 2
    MT = ar.bf16(KC * S, "MT").rearrange("p (k t) -> p k t", k=KC)
    mt_lo = ar.top - KC * S // 2
    ACCF = ar.f32(NT * D, "ACC")
    acc_lo = ar.top - NT * D
    ACC = ACCF.rearrange("p (t d) -> p t d", t=NT)
    GATE = ar.f32(NT * NEXP, "GATE").rearrange("p (t e) -> p t e", t=NT)
    LNG = ar.f32(D, "LNG"); LNB = ar.f32(D, "LNB")
    CCOL = ar.f32(8, "ccol")
    tail_lo = ar.top
    A_XT = Arena(arena_t[:, xt_lo:xt_lo + KC * S // 2], KC * S // 2)
    A_MT = Arena(arena_t[:, mt_lo:mt_lo + KC * S // 2], KC * S // 2)
    A_ACC = Arena(arena_t[:, acc_lo:acc_lo + NT * D], NT * D)
    A_TL = Arena(arena_t[:, tail_lo:NW], NW - tail_lo)
    YT = A_ACC.bf16(12 * S, "YT").rearrange("p (c t) -> p c t", c=12)
    yt_mark = A_ACC.mark()

    ccols = {}

    def constcol(v):
        if v not in ccols:
            i = len(ccols)
            assert i < 8
            ccols[v] = CCOL[:, i:i + 1]
            sc.op("dve", lambda e, i=i, v=v: e.memset(CCOL[:, i:i + 1], float(v)), w=[("ccol", i)])
        return ccols[v]

    C_EPS_RMS = constcol(RMS_EPS)
    C_EPS_LN = constcol(LN_EPS)
    C_EPS_LN2 = constcol(LN_EPS / (ALPHA * ALPHA))
    C_ONE = constcol(1.0)
    SINSC = 2.0 * math.pi * (1.0 - 1e-6)
    C_SINB = constcol(-math.pi * (1.0 - 1e-6))
    CK = [("ccol", i) for i in range(8)]

    def load_ln(g_ap, b_ap):
        sc.dma("sp", lambda e: e.dma_start(out=LNG, in_=g_ap.partition_broadcast(128)), w=["LNG"])
        sc.dma("sp", lambda e: e.dma_start(out=LNB, in_=b_ap.partition_broadcast(128)), w=["LNB"])

    def wview(l, c0, c1):
        return w_in[l].rearrange("(k p) n -> p k n", p=128)[:, :, c0:c1]

    def wtile(arena, ncols, name):
        return arena.bf16(KC * ncols, name).rearrange("p (k n) -> p k n", k=KC)

    def load_w(dst3, src3, key):
        sc.dma("pool", lambda e: e.dma_start(out=dst3, in_=src3), w=[key])

    def xkeys(xkey, blk):
        return [(xkey, blk * 4 + i) for i in range(4)]

    def proj_fm(bank, wt3, ncols, blk, wkey, xt=None, xkey="XT"):
        xt = XT if xt is None else xt
        for kc in range(KC):
            sc.op("pe", lambda e, kc=kc: e.matmul(PS[bank][0:ncols, :], lhsT=wt3[:, kc, 0:ncols],
                                                  rhs=xt[:, kc, blk * 512:(blk + 1) * 512],
                                                  start=(kc == 0), stop=(kc == KC - 1)),
                  r=[wkey] + xkeys(xkey, blk), w=[psk(bank)])

    def proj_tm(bank, c0, wt3, ncols, t, wkey, xt=None, xkey="XT"):
        xt = XT if xt is None else xt
        for kc in range(KC):
            sc.op("pe", lambda e, kc=kc: e.matmul(PS[bank][:, c0:c0 + ncols], lhsT=xt[:, kc, t * 128:(t + 1) * 128],
                                                  rhs=wt3[:, kc, 0:ncols],
                                                  start=(kc == 0), stop=(kc == KC - 1)),
                  r=[wkey, (xkey, t)], w=[psk(bank)])

    def ln_and_xt(tiles, SMALL, XB, dst, dkey, post=None, eps_col=None):
        def sm(i):
            return SMALL[:, i * 16:(i + 1) * 16]
        for i, (t, xa, xk) in enumerate(tiles):
            for hf in range(2):
                sc.op("dve", lambda e, i=i, xa=xa, hf=hf: e.bn_stats(out=sm(i)[:, hf * 6:(hf + 1) * 6], in_=xa[:, hf * 512:(hf + 1) * 512]),
                      r=[xk], w=[("sm", i)])
        for i, (t, xa, xk) in enumerate(tiles):
            sc.op("dve", lambda e, i=i: e.bn_aggr(out=sm(i)[:, 12:14], in_=sm(i)[:, 0:12]), r=[("sm", i)], w=[("sm", i)])
        for i, (t, xa, xk) in enumerate(tiles):
            sc.op("act", lambda e, i=i: e.activation(out=sm(i)[:, 14:15], in_=sm(i)[:, 13:14], func=AF.Sqrt, bias=(C_EPS_LN if eps_col is None else eps_col), scale=1.0),
                  r=[("sm", i)] + CK, w=[("sm", i)])
        for i, (t, xa, xk) in enumerate(tiles):
            sc.op("dve", lambda e, i=i: e.reciprocal(out=sm(i)[:, 14:15], in_=sm(i)[:, 14:15]), r=[("sm", i)], w=[("sm", i)])

        def evac(i):
            t, xa, xk = tiles[i]
            bank = 6 + (i % 2)
            pb = PS[bank].bitcast(BF16)
            sc.op("dve", lambda e: e.tensor_copy(out=dst[:, :, t * 128:(t + 1) * 128], in_=pb.rearrange("p (k c) -> p k c", k=KC)),
                  r=[psk(bank)], w=[(dkey, t)])

        for i, (t, xa, xk) in enumerate(tiles):
            sc.op("dve", lambda e, i=i, xa=xa: e.scalar_tensor_tensor(out=xa, in0=xa, scalar=sm(i)[:, 12:13], in1=LNG, op0=ALU.subtract, op1=ALU.mult),
                  r=[xk, ("sm", i), "LNG"], w=[xk])
            sc.op("dve", lambda e, i=i, xa=xa: e.scalar_tensor_tensor(out=xa, in0=xa, scalar=sm(i)[:, 14:15], in1=LNB, op0=ALU.mult, op1=ALU.add),
                  r=[xk, ("sm", i), "LNB"], w=[xk])
            if post is not None:
                post(t, xa, xk)
            if dst is not None:
                xb = XB[i % 2]
                bank = 6 + (i % 2)
                pb = PS[bank].bitcast(BF16)
                sc.op("act", lambda e, xa=xa, xb=xb: e.activation(out=xb, in_=xa, func=AF.Copy), r=[xk], w=[("xb", i % 2)])
                for kc in range(KC):
                    sc.op("pe", lambda e, kc=kc, xb=xb, pb=pb: e.transpose(out=pb[:, kc * 128:(kc + 1) * 128], in_=xb[:, kc * 128:(kc + 1) * 128], identity=IDB),
                          r=[("xb", i % 2), "CB"], w=[psk(bank)])
                if i >= 1:
                    evac(i - 1)
        if dst is not None:
            evac(len(tiles) - 1)

    def rms_finish(src, srckey, sq, sqkey, rs, rskey, bank):
        sc.op("act", lambda e: e.activation(out=sq, in_=src, func=AF.Square), r=[srckey], w=[sqkey])
        sc.op("pe", lambda e: e.matmul(PS[bank][:, :], lhsT=ONESF, rhs=sq, start=True, stop=True), r=[sqkey, "CF"], w=[psk(bank)])
        sc.op("act", lambda e: e.activation(out=rs, in_=PS[bank][:, :], func=AF.Sqrt, bias=C_EPS_RMS, scale=1.0 / 128.0),
              r=[psk(bank)] + CK, w=[rskey])
        sc.op("dve", lambda e: e.reciprocal(out=rs, in_=rs), r=[rskey], w=[rskey])

    def hgrn_walloc(wk, par):
        return [wtile(wk, 128, "hw%d_%d" % (par, i)) for i in range(4)] + [("hw", par)]

    def hgrn_load(l, h, W):
        for i, o in enumerate((O_HQ, O_HF, O_HI, O_HG)):
            load_w(W[i], wview(l, o + h * 128, o + (h + 1) * 128), W[4])

    def hgrn_unit(l, h, wk, W):
        mk = wk.mark()
        li = l * 4 + h
        WQ, WF, WI, WG, wkey_ = W
        VT = wk.bf16(NT * 128, "vt").rearrange("p (t v) -> p t v", t=NT)
        ST = wk.f32(128, "st")
        KDT = [wk.bf16(128, "kdt%d" % i) for i in range(2)]
        AT = [wk.bf16(128, "at%d" % i) for i in range(2)]
        SG = wk.f32(512, "sg"); QS = wk.f32(512, "qs"); BB = wk.f32(512, "bb"); ENB = wk.f32(512, "enb")
        GSs = [wk.f32(512, "gs%d" % i) for i in range(2)]; SQs = [wk.f32(512, "sq%d" % i) for i in range(2)]
        RSs = [wk.f32(512, "rs%d" % i) for i in range(2)]; EBs = [wk.f32(512, "eb%d" % i) for i in range(2)]
        QTLs = [wk.f32(512, "qtl%d" % i) for i in range(2)]; KTLs = [wk.f32(512, "ktl%d" % i) for i in range(2)]
        KDLs = [wk.bf16(512, "kdl%d" % i) for i in range(2)]
        sc.op("dve", lambda e: e.memset(ST, 0.0), w=["ST"])
        pb4 = PS[5].bitcast(BF16)
        for blk in range(NB):
            bp = blk % 2
            GS = GSs[bp]; SQ = SQs[bp]; RS = RSs[bp]; EB = EBs[bp]; QTL = QTLs[bp]; KTL = KTLs[bp]; KDL = KDLs[bp]
            kGS = ("GS", bp); kSQ = ("SQ", bp); kRS = ("RS", bp); kEB = ("EB", bp); kQ = ("QTL", bp); kK = ("KTL", bp); kKD = ("KDL", bp)
            ob = 6 + bp
            bc = slice(blk * 512, (blk + 1) * 512)
            for tt in range(4):
                proj_tm(3, tt * 128, WI, 128, blk * 4 + tt, wkey_)
            sc.op("act", lambda e, blk=blk: e.activation(out=VT[:, blk * 4:(blk + 1) * 4, :], in_=PS[3][:, :].rearrange("p (t v) -> p t v", t=4), func=AF.Copy),
                  r=[psk(3)], w=["VT"])
            proj_fm(0, WQ, 128, blk, wkey_)
            proj_fm(1, WF, 128, blk, wkey_)
            proj_fm(2, WG, 128, blk, wkey_)
            sc.op("act", lambda e: e.activation(out=SG, in_=PS[1][:, :], func=AF.Sigmoid), r=[psk(1)], w=["SG"])
            sc.op("act", lambda e: e.activation(out=QS, in_=PS[0][:, :], func=AF.Silu), r=[psk(0)], w=["QS"])
            sc.op("act", lambda e, GS=GS: e.activation(out=GS, in_=PS[2][:, :], func=AF.Silu), r=[psk(2)], w=[kGS])
            sc.op("dve", lambda e, EB=EB: e.tensor_scalar(out=EB, in0=SG, scalar1=OML[:, li:li + 1], scalar2=LB[:, li:li + 1], op0=ALU.mult, op1=ALU.add),
                  r=["SG", "SM"], w=[kEB])
            sc.op("act", lambda e, EB=EB: e.activation(out=EB, in_=EB, func=AF.Ln), r=[kEB], w=[kEB])
            sc.op("dve", lambda e, EB=EB: e.tensor_tensor_scan(out=BB, data0=RMASK, data1=EB, initial=0.0, op0=ALU.mult, op1=ALU.add),
                  r=[kEB, "CF"], w=["BB"])
            sc.op("act", lambda e, EB=EB: e.activation(out=EB, in_=BB, func=AF.Exp), r=["BB"], w=[kEB])
            sc.op("act", lambda e: e.activation(out=ENB, in_=BB, func=AF.Exp, scale=-1.0), r=["BB"], w=["ENB"])
            sc.op("dve", lambda e, EB=EB, QTL=QTL: e.tensor_tensor(out=QTL, in0=QS, in1=EB, op=ALU.mult), r=["QS", kEB], w=[kQ])
            sc.op("dve", lambda e: e.tensor_scalar(out=SG, in0=SG, scalar1=NOML[:, li:li + 1], scalar2=OML[:, li:li + 1], op0=ALU.mult, op1=ALU.add),
                  r=["SG", "SM"], w=["SG"])
            sc.op("dve", lambda e, KTL=KTL: e.tensor_tensor(out=KTL, in0=SG, in1=ENB, op=ALU.mult), r=["SG", "ENB"], w=[kK])
            for ch in range(8):
                cs = slice(ch * 64, (ch + 1) * 64)
                sc.op("dve", lambda e, cs=cs, ch=ch, KDL=KDL, KTL=KTL, EB=EB: e.tensor_scalar(out=KDL[:, cs], in0=KTL[:, cs], scalar1=EB[:, ch * 64 + 63:ch * 64 + 64], scalar2=None, op0=ALU.mult),
                      r=[kK, kEB], w=[kKD])
            for tt in range(4):
                T = blk * 4 + tt
                c0 = tt * 128
                p2 = T % 2
                tcol = slice(p2 * 128, (p2 + 1) * 128)
                scol = slice(p2 * 128, (p2 + 1) * 128)
                sc.op("pe", lambda e, c0=c0, tcol=tcol, KDL=KDL: e.transpose(out=pb4[:, tcol], in_=KDL[:, c0:c0 + 128], identity=IDB),
                      r=[kKD, "CB"], w=[("ps5t", p2)])
                sc.op("act", lambda e, p2=p2, tcol=tcol: e.activation(out=KDT[p2], in_=pb4[:, tcol], func=AF.Copy), r=[("ps5t", p2)], w=[("KDT", p2)])
                sc.op("pe", lambda e, c0=c0, scol=scol, KTL=KTL, QTL=QTL: e.matmul(PS[4][:, scol], lhsT=KTL[:, c0:c0 + 128], rhs=QTL[:, c0:c0 + 128], start=True, stop=True),
                      r=[kK, kQ], w=[("ps4s", p2)])
                sc.op("dve", lambda e, p2=p2, scol=scol: e.tensor_tensor(out=AT[p2], in0=PS[4][:, scol], in1=TRIHB, op=ALU.mult), r=[("ps4s", p2), "CB"], w=[("AT", p2)])
                sc.op("pe", lambda e, c0=c0, p2=p2, T=T, ob=ob: e.matmul(PS[ob][:, c0:c0 + 128], lhsT=VT[:, T, :], rhs=AT[p2], start=True, stop=False),
                      r=["VT", ("AT", p2)], w=[psk(ob)])
                for ch in range(2):
                    cc = c0 + ch * 64
                    rows = slice(ch * 64, (ch + 1) * 64)
                    sc.op("pe", lambda e, cc=cc, ch=ch, ob=ob, QTL=QTL: e.matmul(PS[ob][:, cc:cc + 64], lhsT=ST, rhs=QTL[:, cc:cc + 64], start=False, stop=(ch == 1)),
                          r=["ST", kQ], w=[psk(ob)])
                    sc.op("pe", lambda e, rows=rows, p2=p2, T=T: e.matmul(PS[3][:, 0:128], lhsT=KDT[p2][rows, :], rhs=VT[rows, T, :], start=True, stop=True),
                          r=[("KDT", p2), "VT"], w=[psk(3)])
                    sc.op("dve", lambda e, cc=cc, EB=EB: e.scalar_tensor_tensor(out=ST, in0=ST, scalar=EB[:, cc + 63:cc + 64], in1=PS[3][:, 0:128], op0=ALU.mult, op1=ALU.add),
                          r=["ST", kEB, psk(3)], w=["ST"])
            rms_finish(PS[ob][:, :], psk(ob), SQ, kSQ, RS, kRS, 2)
            sc.op("dve", lambda e, ob=ob, SQ=SQ, RS=RS: e.tensor_tensor(out=SQ, in0=PS[ob][:, :], in1=RS, op=ALU.mult), r=[psk(ob), kRS, kSQ], w=[kSQ])
            sc.op("dve", lambda e, bc=bc, SQ=SQ, GS=GS: e.scalar_tensor_tensor(out=YT[:, h, bc], in0=SQ, scalar=HGN[:, l:l + 1], in1=GS, op0=ALU.mult, op1=ALU.mult),
                  r=[kSQ, kGS, "SM"], w=[("YT", h)])
        wk.release(mk)

    def attn_walloc(wk, par, n):
        return [wtile(wk, 128, "aw%d_%d" % (par, i)) for i in range(n)] + [("aw", par)]

    def attn_load(l, kind, u, W):
        if kind == "fox":
            offs = (O_FQ, O_FK, O_FV)
        else:
            offs = (O_DQ, O_DK, O_DV)
        for i, o in enumerate(offs):
            load_w(W[i], wview(l, o + u * 128, o + (u + 1) * 128), W[-1])
        if kind != "fox":
            swv = w_sw[l].rearrange("(k p) n -> p k n", p=128)
            load_w(W[3], swv[:, :, u * 128:(u + 1) * 128], W[-1])
            load_w(W[4], swv[:, :, 512 + u * 128:512 + (u + 1) * 128], W[-1])

    def attn_unit(l, kind, u, wk, W, fox=None, rope=None):
        mk = wk.mark()
        is_fox = kind == "fox"
        WQ, WK, WV = W[0], W[1], W[2]
        wkey_ = W[-1]
        if not is_fox:
            WQS, WKS = W[3], W[4]
            T1s = [wk.f32(512, "t1_%d" % i) for i in range(2)]; T2s = [wk.f32(512, "t2_%d" % i) for i in range(2)]
            O0 = wk.f32(512, "o0"); OD = wk.f32(512, "od"); SQ = wk.f32(512, "asq"); RS = wk.f32(512, "ars")
            CT, STB_ = rope
        QT = wk.bf16(S, "qt"); KT = wk.bf16(S, "kt")
        if is_fox:
            VT2 = wk.bf16(NT * 256, "avt2").rearrange("p (t m v) -> p t m v", t=NT, m=2)
            sc.op("dve", lambda e: e.memset(VT2[:, :, 0, 64:128], 1.0), w=["AVT"])
            sc.op("dve", lambda e: e.memset(VT2[:, :, 1, 0:64], 1.0), w=["AVT"])
        else:
            VT = wk.bf16(NT * 128, "avt").rearrange("p (t v) -> p t v", t=NT)
        PT = [wk.bf16(512, "pt%d" % i) for i in range(3)]
        SBK = (0, 1, 7)
        RD = wk.f32(512, "rd")
        for blk in range(NB):
            bc = slice(blk * 512, (blk + 1) * 512)
            for tt in range(4):
                proj_tm(6, tt * 128, WV, 128, blk * 4 + tt, wkey_)
            pv6 = PS[6][:, :].rearrange("p (t v) -> p t v", t=4)
            if is_fox:
                sc.op("act", lambda e, blk=blk, pv6=pv6: e.activation(out=VT2[:, blk * 4:(blk + 1) * 4, 0, 0:64], in_=pv6[:, :, 0:64], func=AF.Copy), r=[psk(6)], w=["AVT"])
                sc.op("act", lambda e, blk=blk, pv6=pv6: e.activation(out=VT2[:, blk * 4:(blk + 1) * 4, 1, 64:128], in_=pv6[:, :, 64:128], func=AF.Copy), r=[psk(6)], w=["AVT"])
            else:
                sc.op("act", lambda e, blk=blk, pv6=pv6: e.activation(out=VT[:, blk * 4:(blk + 1) * 4, :], in_=pv6, func=AF.Copy), r=[psk(6)], w=["AVT"])
            proj_fm(4, WQ, 128, blk, wkey_)
            proj_fm(5, WK, 128, blk, wkey_)
            if is_fox:
                sc.op("act", lambda e, bc=bc: e.activation(out=QT[:, bc], in_=PS[4][:, :], func=AF.Copy, scale=0.125), r=[psk(4)], w=["QT"])
                sc.op("dve", lambda e, bc=bc: e.tensor_copy(out=KT[:, bc], in_=PS[5][:, :]), r=[psk(5)], w=["KT"])
            else:
                for (ii, bank, WS, wkey, dst, dkey) in ((0, 4, WQS, wkey_, QT, "QT"), (1, 5, WKS, wkey_, KT, "KT")):
                    T1 = T1s[ii]; T2 = T2s[ii]
                    sc.op("dve", lambda e, bank=bank, bc=bc, T1=T1: e.tensor_tensor(out=T1, in0=PS[bank][:, :], in1=CT[:, bc], op=ALU.mult), r=[psk(bank), "ROPE"], w=[("T1", ii)])
                    proj_fm(bank, WS, 128, blk, wkey)
                    sc.op("dve", lambda e, bank=bank, bc=bc, T2=T2: e.tensor_tensor(out=T2, in0=PS[bank][:, :], in1=STB_[:, bc], op=ALU.mult), r=[psk(bank), "ROPE"], w=[("T2", ii)])
                    sc.op("dve", lambda e, dst=dst, bc=bc, T1=T1, T2=T2: e.tensor_tensor(out=dst[:, bc], in0=T1, in1=T2, op=ALU.add), r=[("T1", ii), ("T2", ii)], w=[dkey])
        par = [0]
        for g in range(NB):
            gc = slice(g * 512, (g + 1) * 512)
            for m in range(2):
                base = 64 * m
                rows = slice(base, base + 64)
                drows = slice(64 - base, 128 - base)
                nj = 4 * g + 4
                ab = 2 + 2 * par[0]
                par[0] ^= 1
                head = 2 * u + m

                def score(j, g=g, rows=rows, head=head):
                    c0 = max(0, j - 4 * g) * 128
                    b = j % 3
                    sb = SBK[b]
                    qcols = slice(g * 512 + c0, (g + 1) * 512)
                    sc.op("pe", lambda e: e.matmul(PS[sb][:, c0:512], lhsT=KT[rows, j * 128:(j + 1) * 128], rhs=QT[rows, qcols], start=True, stop=(not is_fox)),
                          r=["KT", "QT"], w=[psk(sb)])
                    if is_fox:
                        sc.op("pe", lambda e: e.matmul(PS[sb][:, c0:512], lhsT=ESELB[0:72, head * 128:(head + 1) * 128], rhs=fox[0][0:72, qcols], start=False, stop=True),
                              r=["CB", "CAUG"], w=[psk(sb)])

                def rest(j, g=g, m=m, nj=nj, ab=ab, head=head):
                    c0 = max(0, j - 4 * g) * 128
                    b = j % 3
                    sb = SBK[b]
                    if is_fox:
                        sc.op("act", lambda e: e.activation(out=PT[b][:, c0:512], in_=PS[sb][:, c0:512], func=AF.Exp, bias=fox[1][:, j, head:head + 1], scale=1.0),
                              r=[psk(sb), "NEGC"], w=[("PT", b)])
                    else:
                        sc.op("act", lambda e: e.activation(out=PT[b][:, c0:512], in_=PS[sb][:, c0:512], func=AF.Exp, scale=0.125), r=[psk(sb)], w=[("PT", b)])
                    if j >= 4 * g:
                        sc.op("dve", lambda e: e.tensor_tensor(out=PT[b][:, c0:c0 + 128], in0=PT[b][:, c0:c0 + 128], in1=TRIB, op=ALU.mult), r=[("PT", b), "CB"], w=[("PT", b)])
                    if is_fox:
                        sc.op("pe", lambda e: e.matmul(PS[ab][:, c0:512], lhsT=VT2[:, j, m, :], rhs=PT[b][:, c0:512], start=(j == 0), stop=(j == nj - 1)),
                              r=["AVT", ("PT", b)], w=[psk(ab)])
                    else:
                        sc.op("pe", lambda e: e.matmul(PS[ab][:, c0:512], lhsT=VT[:, j, :], rhs=PT[b][:, c0:512], start=(j == 0), stop=(j == nj - 1)),
                              r=["AVT", ("PT", b)], w=[psk(ab)])
                        sc.op("pe", lambda e: e.matmul(PS[ab + 1][:, c0:512], lhsT=ONESB, rhs=PT[b][:, c0:512], start=(j == 0), stop=(j == nj - 1)),
                              r=["CB", ("PT", b)], w=[psk(ab + 1)])

                score(0)
                if nj > 1:
                    score(1)
                for j in range(nj):
                    if j + 2 < nj:
                        score(j + 2)
                    rest(j)
                if is_fox:
                    sc.op("dve", lambda e, rows=rows, drows=drows, ab=ab: e.reciprocal(out=RD[rows, :], in_=PS[ab][drows, :]), r=[psk(ab)], w=["RD"])
                    sc.op("dve", lambda e, rows=rows, gc=gc, ab=ab: e.tensor_tensor(out=YT[rows, 4 + u, gc], in0=PS[ab][rows, :], in1=RD[rows, :], op=ALU.mult),
                          r=[psk(ab), "RD"], w=[("YT", 4 + u)])
                else:
                    sc.op("dve", lambda e, ab=ab: e.reciprocal(out=RD, in_=PS[ab + 1][:, :]), r=[psk(ab + 1)], w=["RD"])
                    if m == 0:
                        sc.op("dve", lambda e, ab=ab: e.tensor_tensor(out=O0, in0=PS[ab][:, :], in1=RD, op=ALU.mult), r=[psk(ab), "RD"], w=["O0"])
                    else:
                        sc.op("dve", lambda e, ab=ab: e.tensor_tensor(out=OD, in0=PS[ab][:, :], in1=RD, op=ALU.mult), r=[psk(ab), "RD"], w=["OD"])
                        sc.op("dve", lambda e: e.scalar_tensor_tensor(out=OD, in0=OD, scalar=NLAM[:, l:l + 1], in1=O0, op0=ALU.mult, op1=ALU.add),
                              r=["OD", "O0", "SM"], w=["OD"])
                        rms_finish(OD, "OD", SQ, "ASQ", RS, "ARS", ab + 1)
                        sc.op("dve", lambda e: e.tensor_tensor(out=OD, in0=OD, in1=RS, op=ALU.mult), r=["OD", "ARS"], w=["OD"])
                        sc.op("dve", lambda e, gc=gc: e.tensor_scalar(out=YT[:, 8 + u, gc], in0=OD, scalar1=DNG[:, l:l + 1], scalar2=None, op0=ALU.mult),
                              r=["OD", "SM"], w=[("YT", 8 + u)])
        wk.release(mk)

    def fox_prep(l, wk, wk2):
        WFF = wtile(wk2, 72, "wff")
        sc.op("dve", lambda e: e.memset(WFF, 0.0), w=["wff"])
        for b0 in (0, 32, 64):
            sc.dma("pool", lambda e, b0=b0: e.dma_start(out=WFF[:, :, b0:b0 + 8], in_=wview(l, O_FF, O_FF + 8)), r=["wff"], w=["wff"])
        NCF = wk.f32(S, "ncf")
        CAUG = wk.bf16(S, "caug"); MIDB = wk.bf16(S, "midb"); LOB = wk.bf16(S, "lob")
        R1 = wk.f32(S, "r1")
        NEGC = wk.f32(NT * 8, "negc").rearrange("p (t h) -> p t h", t=NT)
        Z512 = wk2.f32(512, "z512"); E1 = wk2.f32(512, "e1")
        sc.op("dve", lambda e: e.memset(Z512, 0.0), w=["Z512"])
        sc.op("dve", lambda e: e.memset(NCF, 0.0), w=["NCF"])
        R = slice(0, 72)
        for blk in range(NB):
            bc = slice(blk * 512, (blk + 1) * 512)
            proj_fm(0, WFF, 72, blk, "wff")
            sc.op("act", lambda e: e.activation(out=E1[R, :], in_=PS[0][R, :], func=AF.Exp, bias=FB[R, l:l + 1], scale=-1.0), r=[psk(0), "SM"], w=["E1"])
            sc.op("act", lambda e: e.activation(out=E1[R, :], in_=E1[R, :], func=AF.Ln, bias=C_ONE[R, :], scale=1.0), r=["E1"] + CK, w=["E1"])
            init = 0.0 if blk == 0 else NCF[R, blk * 512 - 1:blk * 512]
            sc.op("dve", lambda e, bc=bc, init=init: e.tensor_tensor_scan(out=NCF[R, bc], data0=Z512[R, :], data1=E1[R, :], initial=init, op0=ALU.add, op1=ALU.add),
                  r=["E1", "Z512", "NCF"], w=["NCF"])
        sc.op("dve", lambda e: e.tensor_scalar(out=CAUG[R, :], in0=NCF[R, :], scalar1=-1.0, scalar2=None, op0=ALU.mult), r=["NCF"], w=["CAUG"])
        sc.op("dve", lambda e: e.scalar_tensor_tensor(out=R1[R, :], in0=NCF[R, :], scalar=-1.0, in1=CAUG[R, :], op0=ALU.mult, op1=ALU.subtract),
              r=["NCF", "CAUG"], w=["R1"])
        sc.op("dve", lambda e: e.tensor_copy(out=MIDB[R, :], in_=R1[R, :]), r=["R1"], w=["MIDB"])
        sc.op("dve", lambda e: e.tensor_tensor(out=R1[R, :], in0=R1[R, :], in1=MIDB[R, :], op=ALU.subtract), r=["R1", "MIDB"], w=["R1"])
        sc.op("dve", lambda e: e.tensor_copy(out=LOB[R, :], in_=R1[R, :]), r=["R1"], w=["LOB"])
        sc.op("dve", lambda e: e.tensor_copy(out=CAUG[32:40, :], in_=MIDB[32:40, :]), r=["MIDB", "CAUG"], w=["CAUG"])
        sc.op("dve", lambda e: e.tensor_copy(out=CAUG[64:72, :], in_=LOB[64:72, :]), r=["LOB", "CAUG"], w=["CAUG"])
        for T in range(NT):
            sc.op("pe", lambda e, T=T: e.transpose(out=PS[1][:, T * 8:(T + 1) * 8], in_=NCF[0:8, T * 128:(T + 1) * 128], identity=IDF[0:8, 0:8]),
                  r=["NCF", "CF"], w=[psk(1)])
        sc.op("dve", lambda e: e.tensor_copy(out=NEGC, in_=PS[1][:, 0:NT * 8].rearrange("p (t h) -> p t h", t=NT)), r=[psk(1)], w=["NEGC"])
        return CAUG, NEGC

    def rope_tables(q, wk, CT, ST_):
        POSI = wk.f32(S, "posi").bitcast(I32)
        U = wk.f32(S, "ropeu"); KF = wk.f32(S, "ropek"); KI = wk.f32(S, "ropeki").bitcast(I32)
        sc.dma("sp", lambda e: e.dma_start(out=POSI, in_=pos_in[q].partition_broadcast(128)), w=["POSI"])
        sc.op("dve", lambda e: e.tensor_copy(out=U, in_=POSI), r=["POSI"], w=["U"])
        sc.op("dve", lambda e: e.tensor_scalar(out=U, in0=U, scalar1=MISC[:, 0:1], scalar2=None, op0=ALU.mult), r=["U", "CF"], w=["U"])
        for (dst, off, key) in ((CT, 0.75, "c"), (ST_, 0.5, "s")):
            sc.op("dve", lambda e, dst=dst, off=off: e.tensor_scalar(out=dst, in0=U, scalar1=off, scalar2=None, op0=ALU.add), r=["U"], w=["ROPE"])
            sc.op("dve", lambda e, dst=dst: e.tensor_copy(out=KI, in_=dst), r=["ROPE"], w=["KI"])
            sc.op("dve", lambda e: e.tensor_copy(out=KF, in_=KI), r=["KI"], w=["KF"])
            sc.op("dve", lambda e, dst=dst: e.tensor_tensor(out=dst, in0=dst, in1=KF, op=ALU.subtract), r=["ROPE", "KF"], w=["ROPE"])
            sc.op("dve", lambda e, dst=dst: e.tensor_scalar(out=KF, in0=dst, scalar1=0.0, scalar2=None, op0=ALU.is_lt), r=["ROPE"], w=["KF"])
            sc.op("dve", lambda e, dst=dst: e.tensor_tensor(out=dst, in0=dst, in1=KF, op=ALU.add), r=["ROPE", "KF"], w=["ROPE"])
            sc.op("act", lambda e, dst=dst: e.activation(out=dst, in_=dst, func=AF.Sin, bias=C_SINB, scale=SINSC), r=["ROPE"] + CK, w=["ROPE"])
        sc.op("dve", lambda e: e.tensor_scalar(out=ST_, in0=ST_, scalar1=MISC[:, 1:2], scalar2=None, op0=ALU.mult), r=["ROPE", "CF"], w=["ROPE"])

    def dump_bf16(src3, nchunk, key_fn, wk):
        TMPD = wk.f32(S, "dumptmp")
        for c in range(nchunk):
            sc.op("dve", lambda e, c=c: e.tensor_copy(out=TMPD, in_=src3[:, c, :]), r=[key_fn(c)], w=["TMPD"])
            sc.dma("sp", lambda e, c=c: e.dma_start(out=dbg_out[c * 128:(c + 1) * 128, :], in_=TMPD), r=["TMPD"], w=["dbg"])

    stop = False
    for q in range(NSEQ):
        row0 = q * S
        sc.barrier()
        A_TL.release(0)
        load_ln(ln_in_g, ln_in_b)
        XB = [A_TL.bf16(D, "xb%d" % i) for i in range(2)]
        SMALL = A_TL.f32(16 * NT, "small")
        for t in range(NT):
            xa = ACC[:, t, :]
            sc.dma("sp", lambda e, t=t, xa=xa, row0=row0: e.dma_start(out=xa, in_=x_in[row0 + t * 128:row0 + (t + 1) * 128, :]), w=[("ACC", t)])

        def post0(t, xa, xk):
            sc.dma("sp", lambda e: e.dma_start(out=xres[t * 128:(t + 1) * 128, :], in_=xa), r=[xk], w=[("xres", t)])
        ln_and_xt([(t, ACC[:, t, :], ("ACC", t)) for t in range(NT)], SMALL, XB, XT, "XT", post=post0)

        for l in range(L):
            sc.barrier()
            A_TL.release(0); A_MT.release(0); A_ACC.release(yt_mark)
            CT = A_ACC.f32(S, "ropec"); ST_ = A_ACC.f32(S, "ropes")
            rope_tables(q, A_TL, CT, ST_)
            sc.barrier()
            A_TL.release(0)
            HW = [hgrn_walloc(A_MT if S >= 1024 else A_TL, par) for par in range(2)]
            hgrn_load(l, 0, HW[0])
            for h in range(4):
                if h + 1 < 4:
                    hgrn_load(l, h + 1, HW[(h + 1) % 2])
                hgrn_unit(l, h, A_TL, HW[h % 2])
            sc.barrier()
            A_TL.release(0)
            A_MT.release(0)
            foxp = fox_prep(l, A_MT, A_TL)
            sc.barrier()
            A_TL.release(0)
            AW = [attn_walloc(A_TL, par, 3) for par in range(2)]
            attn_load(l, "fox", 0, AW[0])
            for p in range(4):
                if p + 1 < 4:
                    attn_load(l, "fox", p + 1, AW[(p + 1) % 2])
                attn_unit(l, "fox", p, A_TL, AW[p % 2], fox=foxp)
            sc.barrier()
            A_TL.release(0); A_MT.release(0)
            AW = [attn_walloc(A_TL, par, 5) for par in range(2)]
            attn_load(l, "diff", 0, AW[0])
            for h in range(4):
                if h + 1 < 4:
                    attn_load(l, "diff", h + 1, AW[(h + 1) % 2])
                attn_unit(l, "diff", h, A_TL, AW[h % 2], rope=(CT, ST_))
            if dbg is not None and dbg[0] == "yt":
                sc.barrier()
                A_TL.release(0)
                dump_bf16(YT, 12, lambda c: ("YT", c), A_TL)
                stop = True
                break
            sc.barrier()
            A_TL.release(0); A_MT.release(0)
            WGt = [A_TL.bf16(KC * 3 * 128, "wgt%d" % i).rearrange("p (k r n) -> p k r n", k=KC, r=3) for i in range(2)]
            WBt = [A_TL.bf16(3 * 4 * 128, "wbt%d" % i).rearrange("p (r w n) -> p r w n", r=3, w=4) for i in range(2)]
            SIG = [A_TL.f32(512, "sig%d" % i) for i in range(3)]
            MM = A_TL.f32(512, "mm"); MT2 = A_TL.f32(512, "mt2")
            for dc in range(KC):
                b = dc % 2
                for r in range(3):
                    o = O_GT + r * 1024 + dc * 128
                    load_w(WGt[b][:, :, r, :], wview(l, o, o + 128), ("wgt", b))
                    load_w(WBt[b][:, r, :, :], w_branch[l, r].rearrange("(w p) d -> p w d", p=128)[:, :, dc * 128:(dc + 1) * 128], ("wbt", b))
                for blk in range(NB):
                    bc = slice(blk * 512, (blk + 1) * 512)
                    for r in range(3):
                        proj_fm(r, WGt[b][:, :, r, :], 128, blk, ("wgt", b))
                        sc.op("act", lambda e, r=r: e.activation(out=SIG[r], in_=PS[r][:, :], func=AF.Sigmoid), r=[psk(r)], w=[("SIG", r)])
                        for wc in range(4):
                            sc.op("pe", lambda e, r=r, wc=wc, b=b, bc=bc: e.matmul(PS[3 + r][:, :], lhsT=WBt[b][:, r, wc, :], rhs=YT[:, r * 4 + wc, bc],
                                                                                start=(wc == 0), stop=(wc == 3)),
                                  r=[("wbt", b), ("YT", r * 4 + wc)], w=[psk(3 + r)])
                    sc.op("dve", lambda e: e.tensor_tensor(out=MM, in0=SIG[0], in1=PS[3][:, :], op=ALU.mult), r=[("SIG", 0), psk(3)], w=["MM"])
                    sc.op("dve", lambda e: e.tensor_tensor(out=MT2, in0=SIG[1], in1=PS[4][:, :], op=ALU.mult), r=[("SIG", 1), psk(4)], w=["MT2"])
                    sc.op("dve", lambda e: e.tensor_tensor(out=MM, in0=MM, in1=MT2, op=ALU.add), r=["MM", "MT2"], w=["MM"])
                    sc.op("dve", lambda e: e.tensor_tensor(out=MT2, in0=SIG[2], in1=PS[5][:, :], op=ALU.mult), r=[("SIG", 2), psk(5)], w=["MT2"])
                    sc.op("dve", lambda e, dc=dc, bc=bc: e.tensor_tensor(out=MT[:, dc, bc], in0=MM, in1=MT2, op=ALU.add), r=["MM", "MT2"], w=["MTw"])

            sc.barrier()
            A_TL.release(0); A_ACC.release(0); A_XT.release(0)
            load_ln(ln1_g[l], ln1_b[l])
            big_xt = (KC * S // 2) >= 6144 + 2048
            EW0 = None
            if big_xt:
                EW0 = (wtile(A_XT, 1024, "ewgu0"), A_XT.bf16(4 * 1024, "ewd0").rearrange("p (k n) -> p k n", k=4))

            def load_expert(ex, gu, dn, key):
                load_w(gu[:, :, 0:512], ew_g[l, ex].rearrange("(k p) n -> p k n", p=128), key)
                load_w(gu[:, :, 512:1024], ew_u[l, ex].rearrange("(k p) n -> p k n", p=128), key)
                load_w(dn, ew_d[l, ex].rearrange("(k p) n -> p k n", p=128), key)
            if big_xt:
                load_expert(0, EW0[0], EW0[1], ("ew", 0))
            WO = wtile(A_TL, 1024, "wo")
            load_w(WO, w_out[l].rearrange("(k p) n -> p k n", p=128), "wo")
            WR = wtile(A_TL, 36, "wr")
            load_w(WR[:, :, 0:4], rg_w[l].rearrange("(k p) n -> p k n", p=128), "wr")
            load_w(WR[:, :, 4:36], re_w[l].rearrange("(k p) n -> p k n", p=128), "wr")
            RB = A_TL.f32(36, "rb")
            sc.dma("sp", lambda e: e.dma_start(out=RB[:, 0:4], in_=rg_b[l].partition_broadcast(128)), w=["RB"])
            sc.dma("sp", lambda e: e.dma_start(out=RB[:, 4:36], in_=re_b[l].partition_broadcast(128)), w=["RB"])
            XR = [A_TL.f32(D, "xr%d" % i) for i in range(2)]
            XB = [A_TL.bf16(D, "xb%d" % i) for i in range(2)]
            SMALL = A_TL.f32(16 * NT, "small")
            RWA = A_TL.f32(96 * NT, "rwa")
            for t in range(NT):
                tc_ = slice(t * 128, (t + 1) * 128)
                xa = ACC[:, t, :]
                xr = XR[t % 2]
                sc.dma("sp", lambda e, t=t, xr=xr: e.dma_start(out=xr, in_=xres[t * 128:(t + 1) * 128, :]), r=[("xres", t)], w=[("xr", t % 2)])
                for hf in range(2):
                    bank = (t % 2) * 2 + hf
                    for kc in range(KC):
                        sc.op("pe", lambda e, hf=hf, kc=kc, tc_=tc_, bank=bank: e.matmul(PS[bank][:, :], lhsT=MT[:, kc, tc_], rhs=WO[:, kc, hf * 512:(hf + 1) * 512],
                                                                                     start=(kc == 0), stop=(kc == KC - 1)),
                              r=["wo", ("MT", t), "MTw"], w=[psk(bank)])
                    sc.op("dve", lambda e, hf=hf, xa=xa, xr=xr, bank=bank: e.scalar_tensor_tensor(out=xa[:, hf * 512:(hf + 1) * 512], in0=xr[:, hf * 512:(hf + 1) * 512], scalar=float(ALPHA),
                                                                                              in1=PS[bank][:, :], op0=ALU.mult, op1=ALU.add),
                          r=[("xr", t % 2), psk(bank)], w=[("ACC", t)])

            def post1(t, xa, xk):
                if dbg is not None and dbg[0] == "x1" and l == 0:
                    sc.dma("sp", lambda e: e.dma_start(out=dbg_out[t * 128:(t + 1) * 128, :], in_=xa), r=[xk], w=["dbg"])
            ln_and_xt([(t, ACC[:, t, :], ("ACC", t)) for t in range(NT)], SMALL, XB, MT, "MT", post=post1)
            for t in range(NT):
                tc_ = slice(t * 128, (t + 1) * 128)
                bank = 4 + t // 8
                c0 = (t % 8) * 36
                for kc in range(KC):
                    sc.op("pe", lambda e, kc=kc, tc_=tc_, bank=bank, c0=c0: e.matmul(PS[bank][:, c0:c0 + 36], lhsT=MT[:, kc, tc_], rhs=WR[:, kc, :], start=(kc == 0), stop=(kc == KC - 1)),
                          r=["wr", ("MT", t)], w=[psk(bank)])

            def rw(t, a, b):
                return RWA[:, t * 96 + a:t * 96 + b]

            def stage(fn, eng="dve", extra_r=()):
                for t in range(NT):
                    sc.op(eng, (lambda e, t=t: fn(e, t)), r=[("RW", t)] + list(extra_r(t) if callable(extra_r) else extra_r), w=[("RW", t)])
            LGT = lambda t: rw(t, 0, 36)
            GM = lambda t: rw(t, 36, 37); NGM = lambda t: rw(t, 37, 38); EG = lambda t: rw(t, 38, 42); SE = lambda t: rw(t, 42, 43)
            GP = lambda t: rw(t, 43, 44); OH = lambda t: rw(t, 44, 48); ESL = lambda t: rw(t, 48, 56); MX8 = lambda t: rw(t, 56, 64)
            K1 = lambda t: rw(t, 64, 72); K2 = lambda t: rw(t, 72, 80); DM = lambda t: rw(t, 80, 81); EX = lambda t: rw(t, 81, 82)
            W1 = lambda t: rw(t, 82, 83); W2 = lambda t: rw(t, 83, 84); GL = lambda t: rw(t, 84, 92)
            stage(lambda e, t: e.tensor_tensor(out=LGT(t), in0=PS[4 + t // 8][:, (t % 8) * 36:(t % 8) * 36 + 36], in1=RB, op=ALU.add),
                  extra_r=lambda t: [psk(4 + t // 8), "RB"])
            stage(lambda e, t: e.tensor_reduce(out=GM(t), in_=LGT(t)[:, 0:4], axis=AX.X, op=ALU.max))
            stage(lambda e, t: e.tensor_scalar(out=NGM(t), in0=GM(t), scalar1=-1.0, scalar2=None, op0=ALU.mult))
            stage(lambda e, t: e.activation(out=EG(t), in_=LGT(t)[:, 0:4], func=AF.Exp, bias=NGM(t), scale=1.0), eng="act")
            stage(lambda e, t: e.tensor_reduce(out=SE(t), in_=EG(t), axis=AX.X, op=ALU.add))
            stage(lambda e, t: e.reciprocal(out=GP(t), in_=SE(t)))
            stage(lambda e, t: e.tensor_scalar(out=OH(t), in0=LGT(t)[:, 0:4], scalar1=GM(t), scalar2=None, op0=ALU.is_equal))
            stage(lambda e, t: e.tensor_scalar(out=ESL(t), in0=LGT(t)[:, 4:12], scalar1=OH(t)[:, 0:1], scalar2=None, op0=ALU.mult))
            for g in range(1, 4):
                stage(lambda e, t, g=g: e.scalar_tensor_tensor(out=ESL(t), in0=LGT(t)[:, 4 + 8 * g:12 + 8 * g], scalar=OH(t)[:, g:g + 1], in1=ESL(t), op0=ALU.mult, op1=ALU.add))
            stage(lambda e, t: e.max(out=MX8(t), in_=ESL(t)))
            stage(lambda e, t: e.tensor_scalar(out=K1(t), in0=ESL(t), scalar1=MX8(t)[:, 0:1], scalar2=None, op0=ALU.is_equal))
            stage(lambda e, t: e.tensor_scalar(out=K2(t), in0=ESL(t), scalar1=MX8(t)[:, 1:2], scalar2=None, op0=ALU.is_equal))
            stage(lambda e, t: e.tensor_tensor(out=DM(t), in0=MX8(t)[:, 1:2], in1=MX8(t)[:, 0:1], op=ALU.subtract))
            stage(lambda e, t: e.activation(out=EX(t), in_=DM(t), func=AF.Exp), eng="act")
            stage(lambda e, t: e.tensor_scalar(out=W1(t), in0=EX(t), scalar1=1.0, scalar2=None, op0=ALU.add))
            stage(lambda e, t: e.reciprocal(out=W1(t), in_=W1(t)))
            stage(lambda e, t: e.tensor_tensor(out=W2(t), in0=EX(t), in1=W1(t), op=ALU.mult))
            stage(lambda e, t: e.tensor_tensor(out=W1(t), in0=W1(t), in1=GP(t), op=ALU.mult))
            stage(lambda e, t: e.tensor_tensor(out=W2(t), in0=W2(t), in1=GP(t), op=ALU.mult))
            stage(lambda e, t: e.tensor_scalar(out=GL(t), in0=K1(t), scalar1=W1(t), scalar2=None, op0=ALU.mult))
            stage(lambda e, t: e.scalar_tensor_tensor(out=GL(t), in0=K2(t), scalar=W2(t), in1=GL(t), op0=ALU.mult, op1=ALU.add))
            for g in range(4):
                for t in range(NT):
                    sc.op("dve", lambda e, g=g, t=t: e.tensor_scalar(out=GATE[:, t, g * 8:(g + 1) * 8], in0=GL(t), scalar1=OH(t)[:, g:g + 1], scalar2=float(1.0 / ALPHA), op0=ALU.mult, op1=ALU.mult),
                          r=[("RW", t)], w=["GATE"])
            if dbg is not None and dbg[0] == "x1" and l == 0:
                stop = True
                break

            sc.barrier()
            A_TL.release(0)
            if not big_xt:
                A_XT.release(0)
                EW0 = (wtile(A_TL, 1024, "ewgu0"), A_TL.bf16(4 * 1024, "ewd0").rearrange("p (k n) -> p k n", k=4))
                load_expert(0, EW0[0], EW0[1], ("ew", 0))
            EWGU = [EW0[0], wtile(A_TL, 1024, "ewgu1")]
            EWD = [EW0[1], A_TL.bf16(4 * 1024, "ewd1").rearrange("p (k n) -> p k n", k=4)]
            HA = A_XT
            HT = HA.bf16(4 * 512, "ht").rearrange("p (f t) -> p f t", f=4)
            SGT = [HA.f32(512, "sgt%d" % i) for i in range(2)]
            for ex in range(NEXP):
                b = ex % 2
                if ex > 0:
                    load_expert(ex, EWGU[b], EWD[b], ("ew", b))
                for blk in range(NB):
                    for fc in range(4):
                        bg = (fc % 2) * 2
                        proj_fm(bg, EWGU[b][:, :, fc * 128:(fc + 1) * 128], 128, blk, ("ew", b), xt=MT, xkey="MT")
                        proj_fm(bg + 1, EWGU[b][:, :, 512 + fc * 128:512 + (fc + 1) * 128], 128, blk, ("ew", b), xt=MT, xkey="MT")
                        sc.op("act", lambda e, fc=fc, bg=bg: e.activation(out=SGT[fc % 2], in_=PS[bg][:, :], func=AF.Silu), r=[psk(bg)], w=[("SGT", fc % 2)])
                        sc.op("dve", lambda e, fc=fc, bg=bg: e.tensor_tensor(out=HT[:, fc, :], in0=SGT[fc % 2], in1=PS[bg + 1][:, :], op=ALU.mult),
                              r=[("SGT", fc % 2), psk(bg + 1)], w=["HT"])
                    for tt in range(4):
                        T = blk * 4 + tt
                        for hf in range(2):
                            bank = 4 + (tt * 2 + hf) % 4
                            for fc in range(4):
                                sc.op("pe", lambda e, bank=bank, fc=fc, tt=tt, hf=hf, b=b: e.matmul(PS[bank][:, :], lhsT=HT[:, fc, tt * 128:(tt + 1) * 128],
                                                                                                   rhs=EWD[b][:, fc, hf * 512:(hf + 1) * 512], start=(fc == 0), stop=(fc == 3)),
                                      r=["HT", ("ew", b)], w=[psk(bank)])
                            sc.op("dve", lambda e, bank=bank, T=T, hf=hf, ex=ex: e.scalar_tensor_tensor(out=ACC[:, T, hf * 512:(hf + 1) * 512], in0=PS[bank][:, :],
                                                                                                     scalar=GATE[:, T, ex:ex + 1], in1=ACC[:, T, hf * 512:(hf + 1) * 512],
                                                                                                     op0=ALU.mult, op1=ALU.add),
                                  r=[psk(bank), "GATE", ("ACC", T)], w=[("ACC", T)])

            sc.barrier()
            A_TL.release(0); A_XT.release(0)
            load_ln(ln2_g[l], ln2_b[l])
            XB = [A_TL.bf16(D, "xb%d" % i) for i in range(2)]
            SMALL = A_TL.f32(16 * NT, "small")
            last = (l == L - 1)
            is_dbg2 = dbg is not None and dbg[0] == "x2" and l == 0

            def post2(t, xa, xk, last=last, is_dbg2=is_dbg2, row0=row0):
                if is_dbg2:
                    sc.dma("sp", lambda e: e.dma_start(out=dbg_out[t * 128:(t + 1) * 128, :], in_=xa), r=[xk], w=["dbg"])
                elif last:
                    sc.dma("sp", lambda e: e.dma_start(out=y_out[row0 + t * 128:row0 + (t + 1) * 128, :], in_=xa), r=[xk], w=[("y", t)])
                else:
                    sc.dma("sp", lambda e: e.dma_start(out=xres[t * 128:(t + 1) * 128, :], in_=xa), r=[xk], w=[("xres", t)])
            ln_and_xt([(t, ACC[:, t, :], ("ACC", t)) for t in range(NT)], SMALL, XB, (None if (last or is_dbg2) else XT), "XT", post=post2, eps_col=C_EPS_LN2)
            if dbg is not None and dbg[0] == "x2" and l == 0:
                stop = True
                break
        if stop:
            break

    sc.emit(nc, es)
    es.close()
    return nc


def make_w_sw(w_in):
    L = w_in.shape[0]
    perm64 = np.concatenate([np.arange(8, 16), np.arange(0, 8), np.arange(16, 64)])
    out = np.empty((L, D, 1024), np.float32)
    for i, o in enumerate((O_DQ, O_DK)):
        blk = w_in[:, :, o:o + 512].reshape(L, D, 8, 64)[:, :, :, perm64]
        out[:, :, i * 512:(i + 1) * 512] = blk.reshape(L, D, 512)
    return out


_W_KEYS = ["ln_in_g", "ln_in_b", "w_in", "hgrn_lb_logits", "hgrn_norm_g", "fox_f_bias", "diff_lambda", "diff_norm_g",
           "w_branch", "w_out", "ln1_g", "ln1_b", "router_g_w", "router_g_b", "router_e_w",
           "expert_w_gate", "expert_w_up", "expert_w_down", "ln2_g", "ln2_b"]


def kernel(**inputs):
    x = np.ascontiguousarray(np.asarray(inputs["x"], dtype=np.float32))
    pos = np.ascontiguousarray(np.asarray(inputs["positions"]).astype(np.int32))
    B, S, Dm = x.shape
    ncores = 8
    per = B // ncores
    shared = {k: np.ascontiguousarray(np.asarray(inputs[k], dtype=np.float32)) for k in _W_KEYS}
    shared["router_e_b"] = np.ascontiguousarray(np.asarray(inputs["router_e_b"], dtype=np.float32).reshape(DEPTH, 32))
    shared["w_sw"] = make_w_sw(shared["w_in"])
    shared["consts"] = CONST_ARR
    nc = build_program(S, per)
    in_maps = []
    for c in range(ncores):
        m = dict(shared)
        m["x"] = x[c * per:(c + 1) * per].reshape(per * S, Dm)
        m["pos"] = pos[c * per:(c + 1) * per]
        in_maps.append(m)
    res = run_bass_kernel_spmd(nc, in_maps, core_ids=list(range(ncores)))
    outs = [np.asarray(r["y"], dtype=np.float32).reshape(per, S, Dm) for r in res.results]
    return np.concatenate(outs, axis=0)
```
